# Optimizing a Trainium2 kernel written in Bass

```python
import math
import jax, jax.numpy as jnp
from jax import lax
import numpy as np

D_MODEL = 1024
BATCH = 8
SEQ = 8192
DEPTH = 1

MEM_LEN = 256
EPS = 1e-6

ATT_HEAD_DIM = 64
ATT_WIDTH = D_MODEL // 2
ATT_HEADS = ATT_WIDTH // ATT_HEAD_DIM
DILATED_CONFIGS = ((128, 1), (512, 4), (2048, 16))
ATT_BLOCK = 128
ROPE_THETA = 500000.0
ROPE_DIM = ATT_HEAD_DIM // 4

M_WIDTH = D_MODEL - ATT_WIDTH
M_HEADS = 4
M_HEAD_DIM = M_WIDTH // M_HEADS
CONV_WIDTH = 4
M_CHUNK = 128

IN_WIDTH = 3 * ATT_WIDTH + 3 * M_WIDTH + 2 * M_HEADS
MIX_WIDTH = ATT_WIDTH + M_WIDTH

X_HEADS = 4
X_HEAD_DIM = 64
X_WIDTH = X_HEADS * X_HEAD_DIM

N_GROUPS = 4
EXPERTS_PER_GROUP = 4
N_EXPERTS = N_GROUPS * EXPERTS_PER_GROUP
TOP_K = 2
EXPERT_FF = 512
MOE_BLOCK = 128

kernel_name = 'hymba_longnet_mlstm_hmoe_block'


def rms_norm(x, g):
    xf = x.astype(jnp.float32)
    y = xf * lax.rsqrt(jnp.mean(xf * xf, axis=-1, keepdims=True) + EPS)
    return (y * g.astype(jnp.float32)).astype(x.dtype)


def partial_rotary(t, positions):
    half = ROPE_DIM // 2
    inv_freq = ROPE_THETA ** (-jnp.arange(0, ROPE_DIM, 2, dtype=jnp.float32) / ROPE_DIM)
    ang = positions.astype(jnp.float32)[..., None] * inv_freq
    cos = jnp.cos(ang)[:, :, None, :]
    sin = jnp.sin(ang)[:, :, None, :]
    tr = t[..., :ROPE_DIM].astype(jnp.float32)
    t1, t2 = tr[..., :half], tr[..., half:]
    rot = jnp.concatenate([t1 * cos - t2 * sin, t2 * cos + t1 * sin], axis=-1)
    return jnp.concatenate([rot.astype(t.dtype), t[..., ROPE_DIM:]], axis=-1)


def causal_window_attention(q, k, v, window, blk):
    N, L, H, Dh = q.shape
    assert window <= blk
    nb = -(-L // blk)
    Lp = nb * blk
    pad = ((0, 0), (0, Lp - L), (0, 0), (0, 0))
    qb, kb, vb = (jnp.pad(t, pad).reshape(N, nb, blk, H, Dh) for t in (q, k, v))

    def with_prev(t):
        prev = jnp.pad(t, ((0, 0), (1, 0), (0, 0), (0, 0), (0, 0)))[:, :-1]
        return jnp.concatenate([prev, t], axis=2)

    kk, vv = with_prev(kb), with_prev(vb)
    s = jnp.einsum('nbqhd,nbkhd->nbhqk', qb, kk).astype(jnp.float32) * (Dh ** -0.5)
    qi = jnp.arange(blk)[:, None] + blk
    ki = jnp.arange(2 * blk)[None, :]
    dist = qi - ki
    band = (dist >= 0) & (dist <= window)
    valid_key = (jnp.arange(nb)[:, None, None] * blk + ki[None] - blk) >= 0
    mask = band[None] & valid_key
    s = jnp.where(mask[None, :, None, :, :], s, -jnp.inf)
    lse = jax.nn.logsumexp(s, axis=-1)
    p = jnp.exp(s - lse[..., None])
    o = jnp.einsum('nbhqk,nbkhd->nbqhd', p.astype(v.dtype), vv).astype(jnp.float32)
    o = o.reshape(N, Lp, H, Dh)[:, :L]
    lse = lse.transpose(0, 1, 3, 2).reshape(N, Lp, H)[:, :L]
    return o, lse


def dilated_attention(q, k, v):
    B, S, H, Dh = q.shape
    outs, lses = [], []
    for window, d in DILATED_CONFIGS:
        L = S // d

        def to_sub(t):
            return t.reshape(B, L, d, H, Dh).transpose(0, 2, 1, 3, 4).reshape(B * d, L, H, Dh)

        o, lse = causal_window_attention(to_sub(q), to_sub(k), to_sub(v), window // d, ATT_BLOCK)
        outs.append(o.reshape(B, d, L, H, Dh).transpose(0, 2, 1, 3, 4).reshape(B, S, H, Dh))
        lses.append(lse.reshape(B, d, L, H).transpose(0, 2, 1, 3).reshape(B, S, H))
    w = jax.nn.softmax(jnp.stack(lses, axis=0), axis=0)
    return jnp.einsum('cbsh,cbshd->bshd', w, jnp.stack(outs, axis=0))


def mlstm_chunkwise(q, k, v, i_pre, f_pre):
    B, H, S, Dh = q.shape
    L = M_CHUNK
    nc = S // L
    q, k, v = (t.reshape(B, H, nc, L, Dh) for t in (q, k, v))
    ig = i_pre.reshape(B, H, nc, L)
    lf = jax.nn.log_sigmoid(f_pre).reshape(B, H, nc, L)
    b = jnp.cumsum(lf, axis=-1)
    b_last = b[..., -1]

    g = b_last[..., None] - b + ig
    m_loc = jnp.max(g, axis=-1)
    wk = jnp.exp(g - m_loc[..., None])
    C_loc = jnp.einsum('bhcl,bhcld,bhcle->bhcde', wk, k, v)
    n_loc = jnp.einsum('bhcl,bhcld->bhcd', wk, k)

    def step(carry, inp):
        C, n, m = carry
        Cl, nl, ml, bl = inp
        m_new = jnp.maximum(bl + m, ml)
        a = jnp.exp(bl + m - m_new)
        c = jnp.exp(ml - m_new)
        C_new = a[..., None, None] * C + c[..., None, None] * Cl
        n_new = a[..., None] * n + c[..., None] * nl
        return (C_new, n_new, m_new), (C, n, m)

    init = (jnp.zeros((B, H, Dh, Dh), jnp.float32), jnp.zeros((B, H, Dh), jnp.float32),
            jnp.zeros((B, H), jnp.float32))
    xs = (jnp.moveaxis(C_loc, 2, 0), jnp.moveaxis(n_loc, 2, 0),
          jnp.moveaxis(m_loc, 2, 0), jnp.moveaxis(b_last, 2, 0))
    _, (C_prev, n_prev, m_prev) = lax.scan(step, init, xs)
    C_prev = jnp.moveaxis(C_prev, 0, 2)
    n_prev = jnp.moveaxis(n_prev, 0, 2)
    m_prev = jnp.moveaxis(m_prev, 0, 2)

    causal = jnp.tril(jnp.ones((L, L), dtype=bool))
    Dlog = b[..., :, None] - b[..., None, :] + ig[..., None, :]
    Dlog = jnp.where(causal, Dlog, -jnp.inf)
    m_intra = jnp.max(Dlog, axis=-1)
    m_inter = b + m_prev[..., None]
    m_t = jnp.maximum(m_inter, m_intra)
    Dw = jnp.exp(Dlog - m_t[..., None])
    inter_w = jnp.exp(m_inter - m_t)
    s = jnp.einsum('bhcld,bhcsd->bhcls', q, k) * Dw
    num = jnp.einsum('bhcls,bhcse->bhcle', s, v) + \
        inter_w[..., None] * jnp.einsum('bhcld,bhcde->bhcle', q, C_prev)
    den = jnp.sum(s, axis=-1) + inter_w * jnp.einsum('bhcld,bhcd->bhcl', q, n_prev)
    h = num / jnp.maximum(jnp.abs(den), jnp.exp(-m_t))[..., None]
    return h.reshape(B, H, S, Dh)


def hybrid_mixer(h, positions, w_in, conv_w, conv_b, w_q_m, w_k_m, b_i, b_f, g_mhn, skip_m, w_out):
    B, S, _ = h.shape
    sizes = [ATT_WIDTH] * 3 + [M_WIDTH] * 3 + [M_HEADS, M_HEADS]
    splits = [int(c) for c in np.cumsum(sizes)[:-1]]
    z = h @ w_in
    aq, ak, av, mu, mv, mo, mi, mf = jnp.split(z, splits, axis=-1)

    aq = partial_rotary(aq.reshape(B, S, ATT_HEADS, ATT_HEAD_DIM), positions)
    ak = partial_rotary(ak.reshape(B, S, ATT_HEADS, ATT_HEAD_DIM), positions)
    av = av.reshape(B, S, ATT_HEADS, ATT_HEAD_DIM)
    y_att = dilated_attention(aq, ak, av).reshape(B, S, ATT_WIDTH).astype(h.dtype)

    f32 = jnp.float32
    mu_pad = jnp.pad(mu.astype(f32), ((0, 0), (CONV_WIDTH - 1, 0), (0, 0)))
    c = sum(mu_pad[:, j:j + S] * conv_w[j].astype(f32) for j in range(CONV_WIDTH)) + conv_b.astype(f32)
    c = jax.nn.silu(c)
    ch = c.reshape(B, S, M_HEADS, M_HEAD_DIM)
    mq = jnp.einsum('bshd,hde->bhse', ch, w_q_m.astype(f32))
    mk = jnp.einsum('bshd,hde->bhse', ch, w_k_m.astype(f32)) * (M_HEAD_DIM ** -0.5)
    mvh = mv.astype(f32).reshape(B, S, M_HEADS, M_HEAD_DIM).transpose(0, 2, 1, 3)
    ig = (mi.astype(f32) + b_i.astype(f32)).transpose(0, 2, 1)
    fg = (mf.astype(f32) + b_f.astype(f32)).transpose(0, 2, 1)
    hc = mlstm_chunkwise(mq, mk, mvh, ig, fg).transpose(0, 2, 1, 3)
    hn = hc * lax.rsqrt(jnp.mean(hc * hc, axis=-1, keepdims=True) + EPS)
    hn = hn.reshape(B, S, M_WIDTH) * g_mhn.astype(f32)
    y_m = jax.nn.sigmoid(mo.astype(f32)) * (hn + skip_m.astype(f32) * c)

    y = jnp.concatenate([y_att, y_m.astype(h.dtype)], axis=-1)
    return y @ w_out


def memory_cross_attention(h, mem_n, w_q, w_kv, w_o):
    B, S, _ = h.shape
    M = mem_n.shape[1]
    q = (h @ w_q).reshape(B, S, X_HEADS, X_HEAD_DIM)
    kv = (mem_n @ w_kv).reshape(B, M, 2, X_HEADS, X_HEAD_DIM)
    k, v = kv[:, :, 0], kv[:, :, 1]
    s = jnp.einsum('bshd,bmhd->bhsm', q, k).astype(jnp.float32) * (X_HEAD_DIM ** -0.5)
    p = jax.nn.softmax(s, axis=-1).astype(v.dtype)
    o = jnp.einsum('bhsm,bmhd->bshd', p, v).reshape(B, S, X_WIDTH)
    return o @ w_o


def hierarchical_moe(h, w_rg, b_rg, w_re, b_re, w1, w3, w2):
    N, D = h.shape
    f32 = jnp.float32
    glog = (h @ w_rg).astype(f32) + b_rg.astype(f32)
    gprob = jax.nn.softmax(glog, axis=-1)
    g_idx = jnp.argmax(glog, axis=-1)
    g_w = jnp.take_along_axis(gprob, g_idx[:, None], axis=-1)[:, 0]
    elog = ((h @ w_re).astype(f32) + b_re.astype(f32)).reshape(N, N_GROUPS, EXPERTS_PER_GROUP)
    elog = jnp.take_along_axis(elog, g_idx[:, None, None], axis=1)[:, 0]
    top_v, top_i = lax.top_k(elog, TOP_K)
    e_w = jax.nn.softmax(top_v, axis=-1) * g_w[:, None]
    e_id = g_idx[:, None] * EXPERTS_PER_GROUP + top_i

    A = N * TOP_K
    flat_e = e_id.reshape(A)
    flat_tok = jnp.repeat(jnp.arange(N, dtype=jnp.int32), TOP_K)
    flat_w = e_w.reshape(A)
    order = jnp.argsort(flat_e)
    se, stok, sw = flat_e[order], flat_tok[order], flat_w[order]
    counts = jnp.bincount(flat_e, length=N_EXPERTS)
    starts = jnp.cumsum(counts) - counts
    padded = ((counts + MOE_BLOCK - 1) // MOE_BLOCK) * MOE_BLOCK
    pends = jnp.cumsum(padded)
    pstarts = pends - padded
    dest = pstarts[se] + (jnp.arange(A) - starts[se])
    P = (-(-A // MOE_BLOCK) + N_EXPERTS) * MOE_BLOCK
    nblk = P // MOE_BLOCK
    row_tok = jnp.full((P,), N, dtype=jnp.int32).at[dest].set(stok)
    row_w = jnp.zeros((P,), f32).at[dest].set(sw)
    blk_e = jnp.minimum(jnp.searchsorted(pends, jnp.arange(nblk) * MOE_BLOCK, side='right'),
                        N_EXPERTS - 1)
    h_pad = jnp.concatenate([h, jnp.zeros((1, D), h.dtype)], axis=0)
    xb = h_pad[row_tok].reshape(nblk, MOE_BLOCK, D)

    def expert_block(args):
        xblk, e = args
        return (jax.nn.silu(xblk @ w1[e]) * (xblk @ w3[e])) @ w2[e]

    yb = lax.map(expert_block, (xb, blk_e)).reshape(P, D)
    out = jnp.zeros((N + 1, D), f32).at[row_tok].add(yb.astype(f32) * row_w[:, None])
    return out[:N]


def setup_inputs(seed: int = 0) -> dict:
    key = jax.random.key(seed)
    ks = jax.random.split(key, 32)
    f32 = jnp.float32
    L, D = DEPTH, D_MODEL

    def nrm(k, shape, scale):
        return jax.random.normal(k, shape, f32) * scale

    def gain(k, shape):
        return 1.0 + 0.02 * jax.random.normal(k, shape, f32)

    return {
        'x': nrm(ks[0], (BATCH, SEQ, D), 1.0),
        'mem': nrm(ks[1], (BATCH, MEM_LEN, D), 1.0),
        'positions': jnp.tile(jnp.arange(SEQ, dtype=jnp.int32)[None, :], (BATCH, 1)),
        'g_mix': gain(ks[2], (L, D)),
        'w_in': nrm(ks[3], (L, D, IN_WIDTH), D ** -0.5),
        'conv_w': nrm(ks[4], (L, CONV_WIDTH, M_WIDTH), CONV_WIDTH ** -0.5),
        'conv_b': nrm(ks[5], (L, M_WIDTH), 0.02),
        'w_q_m': nrm(ks[6], (L, M_HEADS, M_HEAD_DIM, M_HEAD_DIM), M_HEAD_DIM ** -0.5),
        'w_k_m': nrm(ks[7], (L, M_HEADS, M_HEAD_DIM, M_HEAD_DIM), M_HEAD_DIM ** -0.5),
        'b_i': nrm(ks[8], (L, M_HEADS), 0.1),
        'b_f': 3.0 + 3.0 * jax.random.uniform(ks[9], (L, M_HEADS), f32),
        'g_mhn': gain(ks[10], (L, M_WIDTH)),
        'skip_m': gain(ks[11], (L, M_WIDTH)),
        'w_out': nrm(ks[12], (L, MIX_WIDTH, D), MIX_WIDTH ** -0.5),
        'g_cross': gain(ks[13], (L, D)),
        'g_mem': gain(ks[14], (L, D)),
        'w_q_x': nrm(ks[15], (L, D, X_WIDTH), D ** -0.5),
        'w_kv_x': nrm(ks[16], (L, D, 2 * X_WIDTH), D ** -0.5),
        'w_o_x': nrm(ks[17], (L, X_WIDTH, D), X_WIDTH ** -0.5),
        'g_ffn': gain(ks[18], (L, D)),
        'w_router_g': nrm(ks[19], (L, D, N_GROUPS), D ** -0.5),
        'b_router_g': nrm(ks[20], (L, N_GROUPS), 0.01),
        'w_router_e': nrm(ks[21], (L, D, N_EXPERTS), D ** -0.5),
        'b_router_e': nrm(ks[22], (L, N_EXPERTS), 0.01),
        'w1': nrm(ks[23], (L, N_EXPERTS, D, EXPERT_FF), D ** -0.5),
        'w3': nrm(ks[24], (L, N_EXPERTS, D, EXPERT_FF), D ** -0.5),
        'w2': nrm(ks[25], (L, N_EXPERTS, EXPERT_FF, D), EXPERT_FF ** -0.5),
        'g_final': gain(ks[26], (D,)),
    }


def reference(x, mem, positions, g_mix, w_in, conv_w, conv_b, w_q_m, w_k_m, b_i, b_f, g_mhn,
              skip_m, w_out, g_cross, g_mem, w_q_x, w_kv_x, w_o_x, g_ffn, w_router_g,
              b_router_g, w_router_e, b_router_e, w1, w3, w2, g_final):
    B, S, D = x.shape
    for l in range(DEPTH):
        h = rms_norm(x, g_mix[l])
        x = x + hybrid_mixer(h, positions, w_in[l], conv_w[l], conv_b[l], w_q_m[l], w_k_m[l],
                             b_i[l], b_f[l], g_mhn[l], skip_m[l], w_out[l]).astype(x.dtype)
        h = rms_norm(x, g_cross[l])
        x = x + memory_cross_attention(h, rms_norm(mem, g_mem[l]), w_q_x[l], w_kv_x[l],
                                       w_o_x[l]).astype(x.dtype)
        h = rms_norm(x, g_ffn[l]).reshape(B * S, D)
        x = x + hierarchical_moe(h, w_router_g[l], b_router_g[l], w_router_e[l], b_router_e[l],
                                 w1[l], w3[l], w2[l]).reshape(B, S, D).astype(x.dtype)
    return rms_norm(x, g_final)
```

```python
from contextlib import ExitStack
import math
import numpy as np
import ml_dtypes
import concourse.bass as bass
import concourse.mybir as mybir
from concourse.bass_utils import run_bass_kernel_spmd

F32 = mybir.dt.float32
BF16 = mybir.dt.bfloat16
I32 = mybir.dt.int32
AF = mybir.ActivationFunctionType
ALU = mybir.AluOpType
AX = mybir.AxisListType

D = 1024
NE = 16
FF = 512
MEM = 256
EPS = 1e-6
NJ = 17
SEG = 20000


class Buf:
    def __init__(self, name, ro=False):
        self.name = name
        self.w = None
        self.r = []
        self.ro = ro


class Sched:
    def __init__(self, nc, stack, tag):
        self.nc, self.stack, self.tag = nc, stack, tag
        self.gate = None
        self.sems = {}
        self.prog = {e: [] for e in ("pe", "act", "dve", "pool", "sp")}
        self.cnt = {e: 0 for e in self.prog}
        self.waited = {e: {} for e in self.prog}
        self.dcnt = {}
        self.deferq = []
        self.defer = False

    def flush(self, n=None):
        k = len(self.deferq) if n is None else min(n, len(self.deferq))
        items, self.deferq = self.deferq[:k], self.deferq[k:]
        for it in items:
            self._op(*it)

    def sem(self, key):
        if key not in self.sems:
            self.sems[key] = self.stack.enter_context(
                self.nc.semaphore("%s_s%d" % (self.tag, len(self.sems))))
        return self.sems[key]

    def _wait(self, eng, ev):
        key, v = ev
        if key[0] == "eng":
            if key[1] == "pe" and eng == "pe":
                return
            k2 = ("eng", key[1])
            g = key[2] * SEG + v
            if self.waited[eng].get(k2, 0) >= g:
                return
            self.waited[eng][k2] = g
        else:
            if self.waited[eng].get(key, 0) >= v:
                return
            self.waited[eng][key] = v
        self.sem(key)
        self.prog[eng].append(("wait", key, v))

    def op(self, eng, fn, reads=(), writes=(), dma=None):
        if self.defer:
            self.deferq.append((eng, fn, tuple(reads), tuple(writes), dma))
            return None
        return self._op(eng, fn, reads, writes, dma)

    def _op(self, eng, fn, reads=(), writes=(), dma=None):
        for b in reads:
            if b.w is not None:
                self._wait(eng, b.w)
        for b in writes:
            if b.w is not None:
                self._wait(eng, b.w)
            for ev in b.r:
                self._wait(eng, ev)
        if dma is not None:
            key = ("dma", id(dma))
            self.dcnt[key] = self.dcnt.get(key, 0) + 1
            ev = (key, 16 * self.dcnt[key])
            self.sem(key)
            self.prog[eng].append(("dma", fn, key))
        else:
            n = self.cnt[eng]
            self.cnt[eng] += 1
            key = ("eng", eng, n // SEG)
            ev = (key, n % SEG + 1)
            self.sem(key)
            self.prog[eng].append(("op", fn, key))
        for b in reads:
            if not b.ro:
                b.r.append(ev)
                if len(b.r) > 64:
                    b.r = b.r[-48:]
        for b in writes:
            b.w = ev
            b.r = []
        return ev

    def finish(self, eng="sp"):
        for key, c in list(self.dcnt.items()):
            self._wait(eng, (key, 16 * c))
        for e in ("pe", "act", "dve", "pool"):
            n = self.cnt[e]
            if n > 0 and e != eng:
                self._wait(eng, (("eng", e, (n - 1) // SEG), (n - 1) % SEG + 1))

    def emit(self):
        nc = self.nc
        sems = self.sems
        prog = self.prog

        def run(name, e):
            for it in prog[name]:
                if it[0] == "wait":
                    e.wait_ge(sems[it[1]], it[2])
                elif it[0] == "op":
                    it[1](e).then_inc(sems[it[2]], 1)
                else:
                    it[1](e).then_inc(sems[it[2]], 16)

        with nc.Block() as block:
            @block.sync
            def _(e):
                run("sp", e)

            @block.scalar
            def _(e):
                run("act", e)

            @block.vector
            def _(e):
                run("dve", e)

            @block.gpsimd
            def _(e):
                run("pool", e)

            @block.tensor
            def _(e):
                run("pe", e)


def build(S, debug=False):
    NCH = S // 128
    SPAN = min(2048, S)
    NSP = S // SPAN
    nc = bass.Bass("TRN2", target_bir_lowering=False)

    def din(name, shape, dt=F32):
        return nc.dram_tensor(name, list(shape), dt, kind="ExternalInput").ap()

    x_d = din("x", [S, D])
    mem_d = din("mem", [MEM, D])
    pos_d = din("pos", [128, NCH], I32)
    w_in_d = din("w_in", [D, 3080])
    w_out_d = din("w_out", [D, D])
    wqm_d = din("w_q_m", [4, 128, 128])
    wkm_d = din("w_k_m", [4, 128, 128])
    wqx_d = din("w_q_x", [D, 256])
    wkv_d = din("w_kv_x", [D, 512])
    wox_d = din("w_o_x", [256, D])
    wr_d = din("w_r", [D, 20])
    w1_d = din("w1", [NE, D, FF])
    w3_d = din("w3", [NE, D, FF])
    w2_d = din("w2", [NE, FF, D])
    gk_d = din("gk", [128, 32])
    sm_d = din("sm", [128, 64])
    gff_d = din("gff", [128, D])
    gfin_d = din("gfin", [128, D])
    cb_d = din("cbf", [128, 128 * 3], BF16)
    cf_d = din("cf32", [128, 128 * 2 + 64])
    am_d = din("amask", [128, NJ * 128], BF16)
    out_d = nc.dram_tensor("out", [S, D], F32, kind="ExternalOutput").ap()
    h3T_d = nc.dram_tensor("h3T_scr", [8, 128, S], BF16, kind="Internal").ap()
    BS = 512
    NBLK = (2 * S) // BS + NE
    CAP = S
    NROWS = NBLK * BS
    h3_d = nc.dram_tensor("h3_scr", [S, D], BF16, kind="Internal").ap()
    wb1_d = nc.dram_tensor("wb1_scr", [8192, 1024], BF16, kind="Internal").ap()
    wb3_d = nc.dram_tensor("wb3_scr", [8192, 1024], BF16, kind="Internal").ap()
    wb2_d = nc.dram_tensor("wb2_scr", [8192, 1024], BF16, kind="Internal").ap()
    w1v = w1_d.rearrange("e (k two) f -> (e k) (two f)", two=2)
    w3v = w3_d.rearrange("e (k two) f -> (e k) (two f)", two=2)
    w2v = w2_d.rearrange("e k f -> (e k) f")
    lg_d = nc.dram_tensor("lg_scr", [S, 20], F32, kind="Internal").ap()
    cnt_d = nc.dram_tensor("cnt_scr", [128, NE], F32, kind="Internal").ap()
    xs_d = nc.dram_tensor("xs_scr", [NROWS, D], BF16, kind="Internal").ap()
    ys_d = nc.dram_tensor("ys_scr", [NROWS, D], BF16, kind="Internal").ap()
    sm2_d = din("sm2", [128, 48])
    if debug:
        dbg2_d = nc.dram_tensor("dbg2", [S, D], F32, kind="ExternalOutput").ap()
        dbg3_d = nc.dram_tensor("dbg3", [S, D], F32, kind="ExternalOutput").ap()

    semstack = ExitStack()
    semstack.__enter__()
    with ExitStack() as st:
        sc = Sched(nc, semstack, "A")

        def sb(name, shape, dt):
            return st.enter_context(nc.sbuf_tensor(name, list(shape), dt))

        def ps(name, shape, dt):
            return st.enter_context(nc.psum_tensor(name, list(shape), dt))

        WIN = sb("WIN", [128, 8, 3080], BF16); bWIN = Buf("WIN", ro=True)
        WOA = sb("WOA", [128, 4, D], BF16); bWOA = Buf("WOA", ro=True)
        WOM = sb("WOM", [128, 4, D], BF16); bWOM = Buf("WOM", ro=True)
        WQX = sb("WQX", [128, 8, 256], BF16); bWQX = Buf("WQX", ro=True)
        WKV = sb("WKV", [128, 8, 512], BF16); bWKV = Buf("WKV", ro=True)
        WOX = sb("WOX", [128, 2, D], BF16); bWOX = Buf("WOX", ro=True)
        WQM = sb("WQM", [128, 4, 128], BF16); bWQM = Buf("WQM", ro=True)
        WKM = sb("WKM", [128, 4, 128], BF16); bWKM = Buf("WKM", ro=True)
        WRH = sb("WRH", [128, 8, 20], BF16); bWRH = Buf("WRH", ro=True)
        WRL = sb("WRL", [128, 8, 20], BF16); bWRL = Buf("WRL", ro=True)
        WR32 = sb("WR32", [128, 8, 20], F32); bWR32 = Buf("WR32")
        WRT = sb("WRT", [128, 8, 20], F32); bWRT = Buf("WRT")
        GK = sb("GK", [128, 32], F32); bGK = Buf("GK", ro=True)
        SM = sb("SM", [128, 64], F32); bSM = Buf("SM", ro=True)
        GFF = sb("GFF", [128, D], F32); bGFF = Buf("GFF", ro=True)
        CB = sb("CB", [128, 384], BF16); bCB = Buf("CB", ro=True)
        CF = sb("CF", [128, 320], F32); bCF = Buf("CF", ro=True)
        AM = sb("AM", [128, NJ * 128], BF16); bAM = Buf("AM", ro=True)
        POSI = sb("POSI", [128, NCH], I32); bPOSI = Buf("POSI", ro=True)
        POSF = sb("POSF", [128, NCH], F32); bPOSF = Buf("POSF", ro=True)
        STG = [sb("STG%d" % i, [128, 1032], F32) for i in range(2)]
        bSTG = [Buf("STG%d" % i) for i in range(2)]
        ident = CB[:, 0:128]; tri_b = CB[:, 128:256]
        tri_f = CF[:, 0:128]; ones_f = CF[:, 128:256]; invE = CF[:, 256:320]
        KTR = sb("KTR", [128, 4, NJ * 128], BF16); bKTR = [Buf("KTR%d" % i) for i in range(NJ)]
        VAR = sb("VAR", [128, NJ, 8, 65], BF16); bVAR = [Buf("VAR%d" % i) for i in range(NJ)]
        XT = [sb("XT%d" % i, [128, D], F32) for i in range(3)]; bXT = [Buf("XT%d" % i) for i in range(3)]
        XN = sb("XN", [128, D], BF16); bXN = Buf("XN")
        HT = sb("HT", [128, 8, 128], BF16); bHT = Buf("HT")
        SCR = sb("SCR", [128, 128], F32); bSCR = Buf("SCR")
        ST = sb("STAT", [128, 64], F32); bST = Buf("STAT")
        ROT = sb("ROT", [128, 6, 64], F32); bROT = Buf("ROT")
        TRG = sb("TRG", [128, 2, 64], F32); bTRG = Buf("TRG")
        ROTI = sb("ROTI", [128, 2, 64], I32); bROTI = Buf("ROTI")
        QR = sb("QR", [128, 512], BF16); bQR = Buf("QR")
        KR = sb("KR", [128, 512], BF16); bKR = Buf("KR")
        QT = sb("QT", [128, 4, 2, 128], BF16); bQT = Buf("QT")
        PT = [sb("PT%d" % i, [128, 512], BF16) for i in range(2)]; bPT = [Buf("PT%d" % i) for i in range(2)]
        RDS = sb("RDS", [128, 8, 1], F32); bRD = Buf("RD")
        RDX = sb("RDX", [128, 4, 1], F32); bRDX = Buf("RDX")
        YAT = sb("YAT", [128, 512], BF16); bYAT = Buf("YAT")
        YA = sb("YA", [128, 4, 128], BF16); bYA = Buf("YA")
        MUB = sb("MUB", [128, 4, 131], F32); bMUB = Buf("MUB")
        CPRE = sb("CPRE", [128, 4, 128], F32); bCPRE = Buf("CPRE")
        CT = sb("CT", [128, 4, 128], BF16); bCT = Buf("CT")
        MQT = sb("MQT", [128, 4, 128], BF16); bMQT = Buf("MQT")
        MKT = sb("MKT", [128, 4, 128], BF16); bMKT = Buf("MKT")
        KG = sb("KG", [128, 4, 128], BF16); bKG = Buf("KG")
        VAM = sb("VAM", [128, 4, 129], BF16); bVAM = Buf("VAM")
        GT = sb("GT", [128, 64], F32); bGT = Buf("GT")
        STL4 = sb("STL4", [128, 4, 128], BF16); bSTLh = [Buf("STL%d" % i) for i in range(4)]
        bCPh = [Buf("CPh%d" % i) for i in range(4)]; bCFSh = [Buf("CFSh%d" % i) for i in range(4)]
        bYMh = [Buf("YMh%d" % i) for i in range(4)]; bSSh = [Buf("SSh%d" % i) for i in range(4)]; bEP = Buf("EP")
        CFS = sb("CFS", [128, 4, 129], F32); bCFS = Buf("CFS")
        CBS = sb("CBS", [128, 4, 129], BF16); bCBS = Buf("CBS")
        HN = sb("HN", [128, 4, 128], BF16); bHN = Buf("HN")
        SGB = sb("SGB", [128, 512], BF16); bSG = Buf("SG")
        YM = sb("YM", [128, 4, 128], BF16); bYM = Buf("YM")
        X2 = sb("X2", [128, D], F32); bX2 = Buf("X2")
        X3 = [sb("X3_0", [128, D], F32)] * 2; bX3 = [Buf("X3_0")] * 2
        H2T = sb("H2T", [128, 8, 128], BF16); bH2T = Buf("H2T")
        QXT = sb("QXT", [64, 4, 128], BF16); bQXT = Buf("QXT")
        KXT = sb("KXT", [64, 4, MEM], BF16); bKXT = Buf("KXT", ro=True)
        VXA = sb("VXA", [128, 2, 4, 65], BF16); bVXA = Buf("VXA", ro=True)
        PXT = sb("PXT", [128, 8, 128], BF16); bPXT = Buf("PXT")
        OXT = sb("OXT", [128, 2, 128], BF16); bOXT = Buf("OXT")
        OXK = sb("OXK", [128, 256], BF16); bOXK = Buf("OXK")
        XN3 = X2; bXN3 = bX2
        HI = XN; bHI = bXN
        LO = sb("LO", [128, D], BF16); bLO = Buf("LO")
        H3T = [sb("H3T0", [128, 8, 128], BF16)] * 2; bH3T = [Buf("H3T0")] * 2
        L3T = H2T; bL3T = bH2T
        RT = sb("RT", [128, 160], F32); bRT = Buf("RT")
        SM2 = sb("SM2", [128, 48], F32); bSM2 = Buf("SM2", ro=True)
        STRI = sb("STRI", [128, 128], BF16); bSTRI = Buf("STRI", ro=True)
        ASG = sb("ASG", [128, NE], BF16); bASG = Buf("ASG")
        BASE = sb("BASE", [128, NE], F32); bBASE = Buf("BASE")
        RK = sb("RK", [128, 48], F32); bRK = Buf("RK")
        DST = sb("DST", [128, 2], I32); bDST = Buf("DST")
        TOK = sb("TOK", [128, 4], F32); bTOK = Buf("TOK")
        bXS = Buf("XSdram")
        MEMX = X2; bMEMX = bX2
        MEMT = sb("MEMT", [128, 8, MEM], BF16); bMEMT = Buf("MEMT")

        CVB = MEMT[:].rearrange("p k m -> p (k m)")
        bCV = [Buf("CV0"), Buf("CV1")]
        cv_i = [0]
        PTR = ps("PTR", [128, 8, 128], BF16); bPTR = Buf("PTR")
        PA = ps("PA", [128, 512], F32); bPA = Buf("PA")
        PB = ps("PB", [128, 512], F32); bPB = Buf("PB")
        PS_ = [ps("PS%d" % i, [128, 512], F32) for i in range(2)]; bPS = [Buf("PS%d" % i) for i in range(2)]
        PO = [ps("PO%d" % i, [128, 512], F32) for i in range(2)]; bPO = [Buf("PO%d" % i) for i in range(2)]
        PM = ps("PM", [128, 512], F32); bPM = Buf("PM")

        op = sc.op

        def ld(dst, src, buf):
            op("sp", lambda e: e.dma_start(out=dst, in_=src), writes=[buf], dma=buf)

        ld(GK[:], gk_d, bGK); ld(SM[:], sm_d, bSM); ld(GFF[:], gff_d, bGFF)
        ld(CB[:], cb_d, bCB); ld(CF[:], cf_d, bCF); ld(AM[:], am_d, bAM); ld(POSI[:], pos_d, bPOSI)
        op("dve", lambda e: e.tensor_copy(out=POSF[:], in_=POSI[:]), reads=[bPOSI], writes=[bPOSF])
        ld(SM2[:], sm2_d, bSM2)
        op("dve", lambda e: e.tensor_tensor(out=STRI[:], in0=CB[:, 128:256], in1=CB[:, 0:128], op=ALU.subtract),
           reads=[bCB], writes=[bSTRI])
        op("dve", lambda e: e.tensor_copy(out=BASE[:], in_=SM2[:, 0:16]), reads=[bSM2], writes=[bBASE])

        stg_i = [0]

        def load_cast(dst, src, parts, cols, scale=None):
            i = stg_i[0] % 2
            stg_i[0] += 1
            op("sp", lambda e: e.dma_start(out=STG[i][0:parts, 0:cols], in_=src), writes=[bSTG[i]], dma=bSTG[i])
            if scale is None:
                op("dve", lambda e: e.tensor_copy(out=dst, in_=STG[i][0:parts, 0:cols]), reads=[bSTG[i]], writes=[bWIN])
            else:
                op("dve", lambda e: e.tensor_scalar(out=dst, in0=STG[i][0:parts, 0:cols], scalar1=scale, scalar2=None,
                                                    op0=ALU.mult), reads=[bSTG[i], bGK], writes=[bWIN])

        for kc in range(8):
            for (c0, cn) in ((0, 1024), (1024, 1024), (2048, 1032)):
                load_cast(WIN[:, kc, c0:c0 + cn], w_in_d[kc * 128:(kc + 1) * 128, c0:c0 + cn], 128, cn, GK[:, kc:kc + 1])
        for h in range(4):
            load_cast(WOA[:, h, :], w_out_d[h * 128:(h + 1) * 128, :], 128, D)
            load_cast(WOM[:, h, :], w_out_d[512 + h * 128:512 + (h + 1) * 128, :], 128, D)
            load_cast(WQM[:, h, :], wqm_d[h], 128, 128)
            load_cast(WKM[:, h, :], wkm_d[h], 128, 128)
        for h in range(2):
            load_cast(WOX[:, h, :], wox_d[h * 128:(h + 1) * 128, :], 128, D)
        for kc in range(8):
            load_cast(WQX[:, kc, :], wqx_d[kc * 128:(kc + 1) * 128, :], 128, 256, GK[:, 8 + kc:9 + kc])
            load_cast(WKV[:, kc, :], wkv_d[kc * 128:(kc + 1) * 128, :], 128, 512, GK[:, 16 + kc:17 + kc])
            ld(WR32[:, kc, :], wr_d[kc * 128:(kc + 1) * 128, :], bWR32)
        for b in (bWOA, bWOM, bWQX, bWKV, bWOX, bWQM, bWKM):
            b.w = bWIN.w
        op("dve", lambda e: e.tensor_copy(out=WRH[:], in_=WR32[:]), reads=[bWR32], writes=[bWRH])
        op("dve", lambda e: e.tensor_tensor(out=WRT[:], in0=WR32[:], in1=WRH[:], op=ALU.subtract),
           reads=[bWR32, bWRH], writes=[bWRT])
        op("dve", lambda e: e.tensor_copy(out=WRL[:], in_=WRT[:]), reads=[bWRT], writes=[bWRL])
        op("pool", lambda e: e.memset(VAR[:], 1.0), writes=bVAR)
        op("pool", lambda e: e.memset(VAM[:], 1.0), writes=[bVAM])
        op("pool", lambda e: e.memset(VXA[:], 1.0), writes=[bVXA])
        op("pool", lambda e: e.memset(MUB[:], 0.0), writes=[bMUB])
        op("pool", lambda e: e.memset(QT[:], 0.0), writes=[bQT])
        op("pool", lambda e: e.memset(CFS[:], 0.0), writes=[bCFS] + bCFSh)
        op("pool", lambda e: e.memset(CBS[:], 0.0), writes=[bCBS])

        def rms_stats(src, bsrc, col):
            op("act", lambda e: e.activation(out=XN[:], in_=src, func=AF.Square, accum_out=ST[:, col:col + 1]),
               reads=[bsrc], writes=[bXN, bST])
            op("dve", lambda e: e.tensor_scalar(out=ST[:, col:col + 1], in0=ST[:, col:col + 1], scalar1=1.0 / D,
                                                scalar2=EPS, op0=ALU.mult, op1=ALU.add), reads=[bST], writes=[bST])
            op("act", lambda e: e.activation(out=ST[:, col:col + 1], in_=ST[:, col:col + 1], func=AF.Ln), reads=[bST], writes=[bST])
            op("act", lambda e: e.activation(out=ST[:, col:col + 1], in_=ST[:, col:col + 1], func=AF.Exp, scale=-0.5),
               reads=[bST], writes=[bST])

        def transpose8(src, bsrc, dst, bdst, n=8, eng="dve"):
            for kc in range(n):
                op("pe", lambda e, kc=kc: e.transpose(out=PTR[:, kc, :], in_=src[:, kc * 128:(kc + 1) * 128],
                                                      identity=ident), reads=[bsrc, bCB], writes=[bPTR])
            if eng == "dve":
                op("dve", lambda e: e.tensor_copy(out=dst, in_=PTR[:, 0:n, :]), reads=[bPTR], writes=[bdst])
            else:
                op("act", lambda e: e.activation(out=dst, in_=PTR[:, 0:n, :], func=AF.Copy), reads=[bPTR], writes=[bdst])

        for mt in range(2):
            ld(MEMX[:], mem_d[mt * 128:(mt + 1) * 128, :], bMEMX)
            rms_stats(MEMX[:], bMEMX, 0)
            op("act", lambda e: e.activation(out=XN[:], in_=MEMX[:], func=AF.Copy, scale=ST[:, 0:1]),
               reads=[bMEMX, bST], writes=[bXN])
            transpose8(XN, bXN, MEMT[:, :, mt * 128:(mt + 1) * 128], bMEMT)
        for h in range(4):
            for kc in range(8):
                op("pe", lambda e, h=h, kc=kc: e.matmul(out=PA[0:64, 0:MEM], lhsT=WKV[:, kc, h * 64:(h + 1) * 64],
                                                        rhs=MEMT[:, kc, :], start=(kc == 0), stop=(kc == 7)),
                   reads=[bWKV, bMEMT], writes=[bPA])
            op("dve", lambda e, h=h: e.tensor_copy(out=KXT[:, h, :], in_=PA[0:64, 0:MEM]), reads=[bPA], writes=[bKXT])
        for mt in range(2):
            for kc in range(8):
                op("pe", lambda e, mt=mt, kc=kc: e.matmul(out=PA[:, 0:256], lhsT=MEMT[:, kc, mt * 128:(mt + 1) * 128],
                                                          rhs=WKV[:, kc, 256:512], start=(kc == 0), stop=(kc == 7)),
                   reads=[bWKV, bMEMT], writes=[bPA])
            op("dve", lambda e, mt=mt: e.tensor_copy(out=VXA[:, mt, :, 0:64],
                                                     in_=PA[:, 0:256].rearrange("p (h d) -> p h d", h=4)),
               reads=[bPA], writes=[bVXA])

        ld(XT[0][:], x_d[0:128, :], bXT[0])
        def do_chunk(c):
            xs = c % 2
            if c + 1 < NCH:
                ld(XT[(c + 1) % 3][:], x_d[(c + 1) * 128:(c + 2) * 128, :], bXT[(c + 1) % 3])
            X = XT[c % 3]; bX = bXT[c % 3]
            slot = c % NJ
            rms_stats(X[:], bX, 0)
            op("act", lambda e: e.activation(out=XN[:], in_=X[:], func=AF.Copy, scale=ST[:, 0:1]),
               reads=[bX, bST], writes=[bXN])
            transpose8(XN, bXN, HT[:], bHT)
            op("dve", lambda e, c=c: e.tensor_scalar(out=ROT[:, 1, :], in0=invE, scalar1=POSF[:, c:c + 1], scalar2=None,
                                                     op0=ALU.mult), reads=[bCF, bPOSF], writes=[bROT])
            op("dve", lambda e: e.tensor_scalar_add(out=ROT[:, 2, :], in0=ROT[:, 1, :], scalar1=0.5 * math.pi),
               reads=[bROT], writes=[bROT])
            op("dve", lambda e: e.tensor_scalar(out=ROTI[:], in0=ROT[:, 1:3, :], scalar1=1.0 / (2 * math.pi), scalar2=None,
                                                op0=ALU.mult), reads=[bROT], writes=[bROTI])
            op("dve", lambda e: e.tensor_copy(out=ROT[:, 3:5, :], in_=ROTI[:]), reads=[bROTI], writes=[bROT])
            op("dve", lambda e: e.scalar_tensor_tensor(out=ROT[:, 1:3, :], in0=ROT[:, 3:5, :], scalar=-2 * math.pi,
                                                       in1=ROT[:, 1:3, :], op0=ALU.mult, op1=ALU.add), reads=[bROT], writes=[bROT])
            op("dve", lambda e: e.tensor_single_scalar(out=ROT[:, 3:5, :], in_=ROT[:, 1:3, :], scalar=math.pi, op=ALU.is_gt),
               reads=[bROT], writes=[bROT])
            op("dve", lambda e: e.scalar_tensor_tensor(out=ROT[:, 1:3, :], in0=ROT[:, 3:5, :], scalar=-2 * math.pi,
                                                       in1=ROT[:, 1:3, :], op0=ALU.mult, op1=ALU.add), reads=[bROT], writes=[bROT])
            op("act", lambda e: e.activation(out=TRG[:], in_=ROT[:, 1:3, :], func=AF.Sin), reads=[bROT], writes=[bTRG])
            sinv = TRG[:, 0, :].rearrange("p (h i) -> p h i", h=8)
            cosv = TRG[:, 1, :].rearrange("p (h i) -> p h i", h=8)

            def proj_tok(pt, bpt, c0, n):
                for kc in range(8):
                    op("pe", lambda e, kc=kc: e.matmul(out=pt[:, 0:n], lhsT=HT[:, kc, :], rhs=WIN[:, kc, c0:c0 + n],
                                                       start=(kc == 0), stop=(kc == 7)), reads=[bHT, bWIN], writes=[bpt])

            def rope(pt, bpt, dst, bdst):
                pv = pt[:, :].rearrange("p (h d) -> p h d", h=8)
                dv = dst[:, :].rearrange("p (h d) -> p h d", h=8)
                r = [ROT[:, 3 + i, :].rearrange("p (h i) -> p h i", h=8) for i in range(3)]
                t1 = pv[:, :, 0:8]; t2 = pv[:, :, 8:16]
                op("dve", lambda e: e.tensor_tensor(out=r[0], in0=t1, in1=cosv, op=ALU.mult), reads=[bpt, bTRG], writes=[bROT])
                op("dve", lambda e: e.tensor_tensor(out=r[1], in0=t2, in1=sinv, op=ALU.mult), reads=[bpt, bTRG], writes=[bROT])
                op("dve", lambda e: e.tensor_tensor(out=dv[:, :, 0:8], in0=r[0], in1=r[1], op=ALU.subtract),
                   reads=[bROT], writes=[bdst])
                op("dve", lambda e: e.tensor_tensor(out=r[0], in0=t2, in1=cosv, op=ALU.mult), reads=[bpt, bTRG], writes=[bROT])
                op("dve", lambda e: e.tensor_tensor(out=r[1], in0=t1, in1=sinv, op=ALU.mult), reads=[bpt, bTRG], writes=[bROT])
                op("dve", lambda e: e.tensor_tensor(out=dv[:, :, 8:16], in0=r[0], in1=r[1], op=ALU.add),
                   reads=[bROT], writes=[bdst])
                op("act", lambda e: e.activation(out=dv[:, :, 16:64], in_=pv[:, :, 16:64], func=AF.Copy),
                   reads=[bpt], writes=[bdst])

            proj_tok(PA, bPA, 0, 512)
            rope(PA, bPA, QR, bQR)
            proj_tok(PB, bPB, 512, 512)
            rope(PB, bPB, KR, bKR)
            for kc in range(4):
                op("pe", lambda e, kc=kc: e.transpose(out=PTR[:, kc, :], in_=QR[:, kc * 128:(kc + 1) * 128], identity=ident),
                   reads=[bQR, bCB], writes=[bPTR])
            op("dve", lambda e: e.tensor_copy(out=QT[0:64, :, 0, :], in_=PTR[0:64, 0:4, :]), reads=[bPTR], writes=[bQT])
            op("act", lambda e: e.activation(out=QT[64:128, :, 1, :], in_=PTR[64:128, 0:4, :], func=AF.Copy), reads=[bPTR], writes=[bQT])
            transpose8(KR, bKR, KTR[:, :, slot * 128:(slot + 1) * 128], bKTR[slot], n=4, eng="act")
            proj_tok(PA, bPA, 1024, 512)
            op("act", lambda e: e.activation(out=VAR[:, slot, :, 0:64], in_=PA[:, :].rearrange("p (h d) -> p h d", h=8),
                                             func=AF.Copy), reads=[bPA], writes=[bVAR[slot]])
            sc.defer = True
            nsub = 64 // NCH
            for (srcv, dstv) in ((w1v, wb1_d), (w3v, wb3_d), (w2v, wb2_d)):
                for sub in range(nsub):
                    l0 = (c * nsub + sub) * 128
                    ci = cv_i[0] % 2
                    cv_i[0] += 1
                    op("sp", lambda e, ci=ci, l0=l0, srcv=srcv: e.dma_start(out=STG[ci][:, 0:1024], in_=srcv[l0:l0 + 128, :]),
                       writes=[bSTG[ci]], dma=bSTG[ci])
                    op("pool", lambda e, ci=ci: e.tensor_copy(out=CVB[:, ci * 1024:(ci + 1) * 1024], in_=STG[ci][:, 0:1024]),
                       reads=[bSTG[ci]], writes=[bCV[ci], bMEMT])
                    op("sp", lambda e, ci=ci, l0=l0, dstv=dstv: e.dma_start(out=dstv[l0:l0 + 128, :],
                                                                          in_=CVB[:, ci * 1024:(ci + 1) * 1024]),
                       reads=[bCV[ci]], dma=bCV[ci])
            for h in range(4):
                for kc in range(8):
                    op("pe", lambda e, h=h, kc=kc: e.matmul(out=PM[:, h * 128:(h + 1) * 128],
                                                            lhsT=WIN[:, kc, 1536 + h * 128:1536 + (h + 1) * 128],
                                                            rhs=HT[:, kc, :], start=(kc == 0), stop=(kc == 7)),
                       reads=[bHT, bWIN], writes=[bPM])
            op("dve", lambda e: e.tensor_copy(out=MUB[:, :, 3:131], in_=PM[:, :].rearrange("p (h t) -> p h t", h=4)),
               reads=[bPM], writes=[bMUB])
            for h in range(4):
                op("dve", lambda e, h=h: e.tensor_scalar(out=CPRE[:, h, :], in0=MUB[:, h, 0:128],
                                                         scalar1=SM[:, 8 + h * 4:9 + h * 4], scalar2=SM[:, 24 + h:25 + h],
                                                         op0=ALU.mult, op1=ALU.add), reads=[bMUB, bSM], writes=[bCPh[h]])
            for j in range(1, 4):
                for h in range(4):
                    op("dve", lambda e, h=h, j=j: e.scalar_tensor_tensor(
                        out=CPRE[:, h, :], in0=MUB[:, h, j:j + 128], scalar=SM[:, 8 + h * 4 + j:9 + h * 4 + j],
                        in1=CPRE[:, h, :], op0=ALU.mult, op1=ALU.add), reads=[bMUB, bSM, bCPh[h]], writes=[bCPh[h]])
            proj_tok(PA, bPA, 2048, 512)
            op("dve", lambda e: e.tensor_copy(out=VAM[:, :, 0:128], in_=PA[:, :].rearrange("p (h d) -> p h d", h=4)),
               reads=[bPA], writes=[bVAM])
            proj_tok(PB, bPB, 3072, 8)
            op("dve", lambda e: e.tensor_tensor(out=GT[:, 0:8], in0=PB[:, 0:8], in1=SM[:, 0:8], op=ALU.add),
               reads=[bPB, bSM], writes=[bGT])
            op("act", lambda e: e.activation(out=GT[:, 8:12], in_=GT[:, 4:8], func=AF.Exp, scale=-1.0),
               reads=[bGT], writes=[bGT])
            op("act", lambda e: e.activation(out=GT[:, 12:16], in_=GT[:, 8:12], func=AF.Ln, bias=1.0),
               reads=[bGT], writes=[bGT])
            op("pe", lambda e: e.matmul(out=PB[:, 16:20], lhsT=tri_f, rhs=GT[:, 12:16], start=True, stop=True),
               reads=[bGT, bCF], writes=[bPB])
            op("pe", lambda e: e.matmul(out=PB[:, 32:36], lhsT=ones_f, rhs=GT[:, 12:16], start=True, stop=True),
               reads=[bGT, bCF], writes=[bPB])
            op("dve", lambda e: e.tensor_copy(out=GT[:, 16:20], in_=PB[:, 16:20]), reads=[bPB], writes=[bGT])
            op("dve", lambda e: e.tensor_copy(out=GT[:, 20:24], in_=PB[:, 32:36]), reads=[bPB], writes=[bGT])
            op("dve", lambda e: e.tensor_tensor(out=GT[:, 40:44], in0=GT[:, 0:4], in1=GT[:, 16:20], op=ALU.add),
               reads=[bGT], writes=[bGT])
            op("dve", lambda e: e.tensor_tensor(out=GT[:, 44:48], in0=GT[:, 40:44], in1=GT[:, 20:24], op=ALU.subtract),
               reads=[bGT], writes=[bGT])
            op("act", lambda e: e.activation(out=GT[:, 24:32], in_=GT[:, 40:48], func=AF.Exp), reads=[bGT], writes=[bGT])
            op("act", lambda e: e.activation(out=GT[:, 32:36], in_=GT[:, 16:20], func=AF.Exp, scale=-1.0),
               reads=[bGT], writes=[bGT])
            op("act", lambda e: e.activation(out=GT[:, 36:40], in_=GT[:, 20:24], func=AF.Exp, scale=-1.0),
               reads=[bGT], writes=[bGT])
            op("dve", lambda e: e.tensor_scalar(out=GT[:, 48:52], in0=GT[:, 28:32], scalar1=128.0 ** -0.5, scalar2=None,
                                                op0=ALU.mult), reads=[bGT], writes=[bGT])
            op("act", lambda e: e.activation(out=CT[:], in_=CPRE[:], func=AF.Silu), reads=bCPh, writes=[bCT])
            op("pool", lambda e: e.tensor_copy(out=MUB[:, :, 0:3], in_=MUB[:, :, 128:131]), reads=[bMUB], writes=[bMUB])
            for h in range(4):
                op("pe", lambda e, h=h: e.matmul(out=PM[:, h * 128:(h + 1) * 128], lhsT=WQM[:, h, :], rhs=CT[:, h, :],
                                                 start=True, stop=True), reads=[bWQM, bCT], writes=[bPM])
            for h in range(4):
                op("pe", lambda e, h=h: e.matmul(out=PA[:, h * 128:(h + 1) * 128], lhsT=WKM[:, h, :], rhs=CT[:, h, :],
                                                 start=True, stop=True), reads=[bWKM, bCT], writes=[bPA])
            for h in range(4):
                op("pe", lambda e, h=h: e.matmul(out=PB[:, h * 128:(h + 1) * 128], lhsT=CT[:, h, :], rhs=WKM[:, h, :],
                                                 start=True, stop=True), reads=[bWKM, bCT], writes=[bPB])
            op("dve", lambda e: e.tensor_copy(out=MQT[:].rearrange("p h t -> p (h t)"), in_=PM[:, :]),
               reads=[bPM], writes=[bMQT])
            op("dve", lambda e: e.tensor_scalar(out=MKT[:].rearrange("p h t -> p (h t)"), in0=PA[:, :], scalar1=128.0 ** -0.5,
                                                scalar2=None, op0=ALU.mult), reads=[bPA], writes=[bMKT])
            for h in range(4):
                op("dve", lambda e, h=h: e.tensor_scalar(out=KG[:, h, :], in0=PB[:, h * 128:(h + 1) * 128],
                                                         scalar1=GT[:, 48 + h:49 + h], scalar2=None, op0=ALU.mult),
                   reads=[bPB, bGT], writes=[bKG])
            for h in range(4):
                op("pe", lambda e, h=h: e.matmul(out=PM[:, h * 128:(h + 1) * 128], lhsT=MKT[:, h, :], rhs=MQT[:, h, :],
                                                 start=True, stop=True), reads=[bMKT, bMQT], writes=[bPM])
            for h in range(4):
                op("dve", lambda e, h=h: e.scalar_tensor_tensor(out=STL4[:, h, :], in0=PM[:, h * 128:(h + 1) * 128],
                                                                scalar=GT[:, 24 + h:25 + h], in1=tri_b, op0=ALU.mult, op1=ALU.mult),
                   reads=[bPM, bGT, bCB], writes=[bSTLh[h]])
            for h in range(4):
                op("pe", lambda e, h=h: e.matmul(out=PA[:, h * 128:(h + 1) * 128], lhsT=STL4[:, h, :], rhs=VAM[:, h, 0:128],
                                                 start=True, stop=False), reads=[bSTLh[h], bVAM], writes=[bPA])
                op("pe", lambda e, h=h: e.matmul(out=PA[:, h * 128:(h + 1) * 128], lhsT=MQT[:, h, :], rhs=CBS[:, h, 0:128],
                                                 start=False, stop=True), reads=[bMQT, bCBS], writes=[bPA])
            for h in range(4):
                op("pe", lambda e, h=h: e.matmul(out=PB[:, h:h + 1], lhsT=STL4[:, h, :], rhs=VAM[:, h, 128:129],
                                                 start=True, stop=False), reads=[bSTLh[h], bVAM], writes=[bPB])
                op("pe", lambda e, h=h: e.matmul(out=PB[:, h:h + 1], lhsT=MQT[:, h, :], rhs=CBS[:, h, 128:129],
                                                 start=False, stop=True), reads=[bMQT, bCBS], writes=[bPB])
            for h in range(4):
                op("pe", lambda e, h=h: e.matmul(out=PM[:, h * 128:(h + 1) * 128], lhsT=KG[:, h, :], rhs=VAM[:, h, 0:128],
                                                 start=True, stop=True), reads=[bKG, bVAM], writes=[bPM])
            for h in range(4):
                op("pe", lambda e, h=h: e.matmul(out=PB[:, 8 + h:9 + h], lhsT=KG[:, h, :], rhs=VAM[:, h, 128:129],
                                                 start=True, stop=True), reads=[bKG, bVAM], writes=[bPB])
            for h in range(4):
                op("dve", lambda e, h=h: e.scalar_tensor_tensor(out=CFS[:, h, 0:128], in0=CFS[:, h, 0:128],
                                                                scalar=GT[:, 36 + h:37 + h], in1=PM[:, h * 128:(h + 1) * 128],
                                                                op0=ALU.mult, op1=ALU.add),
                   reads=[bCFSh[h], bGT, bPM], writes=[bCFSh[h]])
            for h in range(4):
                op("dve", lambda e, h=h: e.scalar_tensor_tensor(out=CFS[:, h, 128:129], in0=CFS[:, h, 128:129],
                                                                scalar=GT[:, 36 + h:37 + h], in1=PB[:, 8 + h:9 + h],
                                                                op0=ALU.mult, op1=ALU.add),
                   reads=[bCFSh[h], bGT, bPB], writes=[bCFSh[h]])
            op("dve", lambda e: e.tensor_copy(out=CBS[:], in_=CFS[:]), reads=bCFSh, writes=[bCBS])
            op("dve", lambda e: e.tensor_tensor(out=ST[:, 8:12], in0=PB[:, 0:4], in1=GT[:, 32:36], op=ALU.mult),
               reads=[bPB, bGT], writes=[bEP])
            op("dve", lambda e: e.tensor_scalar(out=ST[:, 12:16], in0=ST[:, 8:12], scalar1=-1.0, scalar2=None, op0=ALU.mult),
               reads=[bEP], writes=[bEP])
            op("dve", lambda e: e.tensor_tensor(out=ST[:, 8:12], in0=ST[:, 8:12], in1=ST[:, 12:16], op=ALU.max),
               reads=[bEP], writes=[bEP])
            op("dve", lambda e: e.tensor_scalar_max(out=ST[:, 8:12], in0=ST[:, 8:12], scalar1=1.0), reads=[bEP], writes=[bEP])
            op("dve", lambda e: e.reciprocal(out=ST[:, 16:20], in_=ST[:, 8:12]), reads=[bEP], writes=[bEP])
            op("dve", lambda e: e.tensor_tensor(out=ST[:, 16:20], in0=ST[:, 16:20], in1=GT[:, 32:36], op=ALU.mult),
               reads=[bEP, bGT], writes=[bEP])
            for h in range(4):
                op("act", lambda e, h=h: e.activation(out=SCR[:, 0:128], in_=PA[:, h * 128:(h + 1) * 128], func=AF.Square,
                                                      accum_out=ST[:, 20 + h:21 + h]), reads=[bPA], writes=[bSCR, bSSh[h]])
            op("dve", lambda e: e.tensor_tensor(out=ST[:, 24:28], in0=ST[:, 16:20], in1=ST[:, 16:20], op=ALU.mult),
               reads=[bEP], writes=[bEP])
            op("dve", lambda e: e.tensor_tensor(out=ST[:, 24:28], in0=ST[:, 24:28], in1=ST[:, 20:24], op=ALU.mult),
               reads=[bEP] + bSSh, writes=[bEP])
            op("dve", lambda e: e.tensor_scalar(out=ST[:, 24:28], in0=ST[:, 24:28], scalar1=1.0 / 128, scalar2=EPS,
                                                op0=ALU.mult, op1=ALU.add), reads=[bEP], writes=[bEP])
            op("act", lambda e: e.activation(out=ST[:, 24:28], in_=ST[:, 24:28], func=AF.Ln), reads=[bEP], writes=[bEP])
            op("act", lambda e: e.activation(out=ST[:, 24:28], in_=ST[:, 24:28], func=AF.Exp, scale=-0.5),
               reads=[bEP], writes=[bEP])
            op("dve", lambda e: e.tensor_tensor(out=ST[:, 24:28], in0=ST[:, 24:28], in1=ST[:, 16:20], op=ALU.mult),
               reads=[bEP], writes=[bEP])
            for h in range(4):
                op("dve", lambda e, h=h: e.tensor_scalar(out=HN[:, h, :], in0=PA[:, h * 128:(h + 1) * 128],
                                                         scalar1=ST[:, 24 + h:25 + h], scalar2=None, op0=ALU.mult),
                   reads=[bPA, bEP], writes=[bHN])
            for h in range(4):
                op("pe", lambda e, h=h: e.transpose(out=PTR[:, h, :], in_=HN[:, h, :], identity=ident),
                   reads=[bHN, bCB], writes=[bPTR])
            for h in range(4):
                for kc in range(8):
                    op("pe", lambda e, h=h, kc=kc: e.matmul(out=PM[:, h * 128:(h + 1) * 128],
                                                            lhsT=WIN[:, kc, 2560 + h * 128:2560 + (h + 1) * 128],
                                                            rhs=HT[:, kc, :], start=(kc == 0), stop=(kc == 7)),
                       reads=[bHT, bWIN], writes=[bPM])
            for h in range(4):
                op("dve", lambda e, h=h: e.tensor_scalar(out=YM[:, h, :], in0=CT[:, h, :], scalar1=SM[:, 32 + h:33 + h], scalar2=None,
                                                         op0=ALU.mult), reads=[bCT, bSM], writes=[bYMh[h]])
            for h in range(4):
                op("dve", lambda e, h=h: e.scalar_tensor_tensor(out=YM[:, h, :], in0=PTR[:, h, :], scalar=SM[:, 28 + h:29 + h],
                                                                in1=YM[:, h, :], op0=ALU.mult, op1=ALU.add),
                   reads=[bPTR, bSM, bYMh[h]], writes=[bYMh[h]])
            op("act", lambda e: e.activation(out=SGB[:], in_=PM[:, :], func=AF.Sigmoid), reads=[bPM], writes=[bSG])
            op("dve", lambda e: e.tensor_tensor(out=YM[:].rearrange("p h t -> p (h t)"), in0=YM[:].rearrange("p h t -> p (h t)"),
                                                in1=SGB[:], op=ALU.mult), reads=bYMh + [bSG], writes=[bYM] + bYMh)
            sc.defer = False
            nj = min(c, NJ - 1) + 1
            groups = []
            for h in range(8):
                js = list(range(nj))
                for g0 in range(0, nj, 4):
                    groups.append((h, js[g0:g0 + 4]))

            def rec_S(gi):
                h, jl = groups[gi]
                pb_ = gi % 2
                po = (h % 2) * 64
                n_ = len(jl) * 128
                j0_ = jl[0]
                op("pe", lambda e: e.matmul(out=PS_[pb_][:, 0:n_], lhsT=ident, rhs=AM[:, j0_ * 128:j0_ * 128 + n_],
                                            start=True, stop=False), reads=[bCB, bAM], writes=[bPS[pb_]])
                for jj, j in enumerate(jl):
                    ks = (c - j) % NJ
                    op("pe", lambda e, jj=jj, ks=ks: e.matmul(
                        out=PS_[pb_][:, jj * 128:(jj + 1) * 128],
                        lhsT=KTR[:, h // 2, ks * 128:(ks + 1) * 128],
                        rhs=QT[:, h // 2, h % 2, :], start=False, stop=(jj == len(jl) - 1)),
                       reads=[bKTR[ks], bQT], writes=[bPS[pb_]])

            def rec_EM(gi):
                h, jl = groups[gi]
                pb_ = gi % 2
                n = len(jl) * 128
                j0 = jl[0]
                op("act", lambda e: e.activation(out=PT[pb_][:, 0:n], in_=PS_[pb_][:, 0:n], func=AF.Exp, scale=0.125),
                   reads=[bPS[pb_]], writes=[bPT[pb_]])

            def rec_PV(gi):
                h, jl = groups[gi]
                pb_ = gi % 2
                for jj, j in enumerate(jl):
                    ks = (c - j) % NJ
                    op("pe", lambda e, jj=jj, ks=ks, j=j: e.matmul(
                        out=PO[h // 4][:, (h % 4) * 65:(h % 4 + 1) * 65],
                        lhsT=PT[pb_][:, jj * 128:(jj + 1) * 128], rhs=VAR[:, ks, h, :],
                        start=(j == 0), stop=(j == nj - 1)),
                       reads=[bVAR[ks], bPT[pb_]], writes=[bPO[h // 4]])

            per = -(-len(sc.deferq) // len(groups))
            rec_S(0)
            for gi in range(len(groups)):
                if gi + 1 < len(groups):
                    rec_S(gi + 1)
                rec_EM(gi)
                rec_PV(gi)
                sc.flush(per)
            sc.flush()
            for hh in range(2):
                op("dve", lambda e, hh=hh: e.reciprocal(
                    out=RDS[:, hh * 4:(hh + 1) * 4, :],
                    in_=PO[hh][:, 0:260].rearrange("p (h d) -> p h d", h=4)[:, :, 64:65]), reads=[bPO[hh]], writes=[bRD])
            for h in range(8):
                op("act", lambda e, h=h: e.activation(out=YAT[:, h * 64:(h + 1) * 64],
                                                      in_=PO[h // 4][:, (h % 4) * 65:(h % 4) * 65 + 64], func=AF.Copy,
                                                      scale=RDS[:, h, :]), reads=[bPO[h // 4], bRD], writes=[bYAT])
            transpose8(YAT, bYAT, YA[:], bYA, n=4)

            sc.defer = True
            for half, (pt, bpt) in enumerate(((PA, bPA), (PB, bPB))):
                cs = slice(half * 512, (half + 1) * 512)
                for h in range(4):
                    op("pe", lambda e, h=h, pt=pt, cs=cs: e.matmul(out=pt[:, :], lhsT=YA[:, h, :], rhs=WOA[:, h, cs],
                                                                  start=(h == 0), stop=False), reads=[bYA, bWOA], writes=[bpt])
                for h in range(4):
                    op("pe", lambda e, h=h, pt=pt, cs=cs: e.matmul(out=pt[:, :], lhsT=YM[:, h, :], rhs=WOM[:, h, cs],
                                                                  start=False, stop=(h == 3)), reads=[bYM, bWOM], writes=[bpt])
                op("dve", lambda e, pt=pt, cs=cs: e.tensor_tensor(out=X2[:, cs], in0=pt[:, :], in1=X[:, cs], op=ALU.add),
                   reads=[bpt, bX], writes=[bX2])
            if debug:
                op("sp", lambda e, c=c: e.dma_start(out=dbg2_d[c * 128:(c + 1) * 128, :], in_=X2[:]), reads=[bX2], dma=bX2)
            rms_stats(X2[:], bX2, 1)
            op("dve", lambda e: e.tensor_scalar(out=XN[:], in0=X2[:], scalar1=ST[:, 1:2], scalar2=None, op0=ALU.mult),
               reads=[bX2, bST], writes=[bXN])
            transpose8(XN, bXN, H2T[:], bH2T)
            for h in range(4):
                for kc in range(8):
                    op("pe", lambda e, h=h, kc=kc: e.matmul(out=PM[0:64, h * 128:(h + 1) * 128],
                                                            lhsT=WQX[:, kc, h * 64:(h + 1) * 64], rhs=H2T[:, kc, :],
                                                            start=(kc == 0), stop=(kc == 7)), reads=[bWQX, bH2T], writes=[bPM])
            op("dve", lambda e: e.tensor_copy(out=QXT[:].rearrange("p h t -> p (h t)"), in_=PM[0:64, :]),
               reads=[bPM], writes=[bQXT])
            for h in range(4):
                for mt in range(2):
                    i = h * 2 + mt
                    op("pe", lambda e, h=h, mt=mt, i=i: e.matmul(out=(PA, PB)[i // 4][:, (i % 4) * 128:(i % 4 + 1) * 128],
                                                                 lhsT=KXT[:, h, mt * 128:(mt + 1) * 128], rhs=QXT[:, h, :],
                                                                 start=True, stop=True), reads=[bKXT, bQXT], writes=[(bPA, bPB)[i // 4]])
            for b2 in range(2):
                op("act", lambda e, b2=b2: e.activation(out=PXT[:, b2 * 4:(b2 + 1) * 4, :].rearrange("p a t -> p (a t)"),
                                                        in_=(PA, PB)[b2][:, :], func=AF.Exp, scale=0.125),
                   reads=[(bPA, bPB)[b2]], writes=[bPXT])
            for h in range(4):
                for mt in range(2):
                    op("pe", lambda e, h=h, mt=mt: e.matmul(out=PM[:, h * 65:(h + 1) * 65], lhsT=PXT[:, h * 2 + mt, :],
                                                            rhs=VXA[:, mt, h, :], start=(mt == 0), stop=(mt == 1)),
                       reads=[bVXA, bPXT], writes=[bPM])
            op("dve", lambda e: e.reciprocal(out=RDX[:, 0:4, :],
                                             in_=PM[:, 0:260].rearrange("p (h d) -> p h d", h=4)[:, :, 64:65]),
               reads=[bPM], writes=[bRDX])
            for h in range(4):
                op("dve", lambda e, h=h: e.tensor_scalar(out=OXK[:, h * 64:(h + 1) * 64], in0=PM[:, h * 65:h * 65 + 64],
                                                         scalar1=RDX[:, h, :], scalar2=None, op0=ALU.mult),
                   reads=[bPM, bRDX], writes=[bOXK])
            transpose8(OXK, bOXK, OXT[:], bOXT, n=2)
            X3c = X3[xs]; bX3c = bX3[xs]
            for half, (pt, bpt) in enumerate(((PA, bPA), (PB, bPB))):
                cs = slice(half * 512, (half + 1) * 512)
                for h in range(2):
                    op("pe", lambda e, h=h, pt=pt, cs=cs: e.matmul(out=pt[:, :], lhsT=OXT[:, h, :], rhs=WOX[:, h, cs],
                                                                  start=(h == 0), stop=(h == 1)), reads=[bOXT, bWOX], writes=[bpt])
                op("dve", lambda e, pt=pt, cs=cs: e.tensor_tensor(out=X3c[:, cs], in0=pt[:, :], in1=X2[:, cs], op=ALU.add),
                   reads=[bpt, bX2], writes=[bX3c])
            op("sp", lambda e, c=c: e.dma_start(out=out_d[c * 128:(c + 1) * 128, :], in_=X3c[:]), reads=[bX3c], dma=bX3c)
            if debug:
                op("sp", lambda e, c=c: e.dma_start(out=dbg3_d[c * 128:(c + 1) * 128, :], in_=X3c[:]), reads=[bX3c], dma=bX3c)
            rms_stats(X3c[:], bX3c, 2)
            op("dve", lambda e: e.scalar_tensor_tensor(out=XN3[:], in0=X3c[:], scalar=ST[:, 2:3], in1=GFF[:], op0=ALU.mult,
                                                       op1=ALU.mult), reads=[bX3c, bST, bGFF], writes=[bXN3])
            op("dve", lambda e: e.tensor_copy(out=HI[:], in_=XN3[:]), reads=[bXN3], writes=[bHI])
            op("dve", lambda e: e.tensor_tensor(out=LO[:], in0=XN3[:], in1=HI[:], op=ALU.subtract),
               reads=[bXN3, bHI], writes=[bLO])
            transpose8(HI, bHI, H3T[xs][:], bH3T[xs])
            transpose8(LO, bLO, L3T[:], bL3T)
            op("sp", lambda e, c=c: e.dma_start(out=h3T_d[:, :, c * 128:(c + 1) * 128].rearrange("k p t -> p k t"),
                                                in_=H3T[xs][:]), reads=[bH3T[xs]], dma=bH3T[xs])
            n_mm = 0
            for (lt, blt, wt, bwt) in ((H3T[xs], bH3T[xs], WRH, bWRH), (L3T, bL3T, WRH, bWRH), (H3T[xs], bH3T[xs], WRL, bWRL)):
                for kc in range(8):
                    op("pe", lambda e, lt=lt, wt=wt, kc=kc, n_mm=n_mm: e.matmul(out=PM[:, 0:20], lhsT=lt[:, kc, :], rhs=wt[:, kc, :],
                                                                              start=(n_mm == 0), stop=(n_mm == 23)),
                       reads=[blt, bwt], writes=[bPM])
                    n_mm += 1
            op("dve", lambda e: e.tensor_tensor(out=RT[:, 0:20], in0=PM[:, 0:20], in1=SM[:, 36:56], op=ALU.add),
               reads=[bPM, bSM], writes=[bRT])
            op("sp", lambda e, c=c: e.dma_start(out=lg_d[c * 128:(c + 1) * 128, :], in_=RT[:, 0:20]), reads=[bRT], dma=bRT)
            op("sp", lambda e, c=c: e.dma_start(out=h3_d[c * 128:(c + 1) * 128, :], in_=HI[:]), reads=[bHI], dma=bHI)
            sc.defer = False

        for c in range(NCH):
            do_chunk(c)
        sc.flush()
        sc.finish("sp")
        sc.emit()

    with ExitStack() as st:
        sc = Sched(nc, semstack, "B")
        op = sc.op

        def sb(name, shape, dt):
            return st.enter_context(nc.sbuf_tensor(name, list(shape), dt))

        def ps(name, shape, dt):
            return st.enter_context(nc.psum_tensor(name, list(shape), dt))

        GFN = sb("bGFN", [128, D], F32); bGFN = Buf("bGFN", ro=True)
        SM2 = sb("bSM2", [128, 48], F32); bSM2 = Buf("bSM2", ro=True)
        IDN = sb("bIDN", [128, 128], BF16); bIDN = Buf("bIDN", ro=True)
        CNT = sb("bCNT", [128, NE], F32); bCNT = Buf("bCNT")
        NBf = sb("bNB", [128, NE], F32); bNB = Buf("bNB")
        NBi = sb("bNBi", [128, NE], I32); bNBi = Buf("bNBi")
        CUM = sb("bCUM", [128, NE], F32); bCUM = Buf("bCUM")
        MSK = sb("bMSK", [128, 2 * NE], F32); bMSK = Buf("bMSK")
        EJ = sb("bEJ", [128, NBLK], F32); bEJ = Buf("bEJ")
        IJ = sb("bIJ", [128, NBLK], F32); bIJ = Buf("bIJ")
        PST = sb("bPST", [128, NE], F32); bPST = Buf("bPST")
        DPF = sb("bDPF", [128, NCH, 2], F32); bDPF = Buf("bDPF")
        DPI = sb("bDPI", [128, NCH, 2], I32); bDPI = Buf("bDPI")
        WAB = sb("bWAB", [128, NCH, 2], F32); bWAB = Buf("bWAB")
        DEC = sb("bDEC", [128, 16], F32); bDEC = Buf("bDEC")
        DECI = sb("bDECI", [128, 2], I32); bDECI = Buf("bDECI")
        H3L = [sb("bH3L%d" % i, [128, D], BF16) for i in range(2)]; bH3L = [Buf("bH3L%d" % i) for i in range(2)]
        EW = sb("bEW", [128, 2, NBLK], F32); bEW = Buf("bEW")
        IXF = sb("bIXF", [128, NBLK, 16], F32); bIXF = Buf("bIXF")
        IXI = sb("bIXI", [128, NBLK, 16], I32); bIXI = Buf("bIXI", ro=True)
        W1 = [sb("bW1_%d" % i, [128, 8, FF], BF16) for i in range(2)]
        W3 = [sb("bW3_%d" % i, [128, 8, FF], BF16) for i in range(2)]
        W2 = [sb("bW2_%d" % i, [128, 4, D], BF16) for i in range(2)]
        bW = [Buf("bW%d" % i) for i in range(2)]
        XB = [sb("bXB%d" % i, [128, 4, D], BF16) for i in range(2)]; bXB = [Buf("bXB%d" % i) for i in range(2)]
        XBT = [sb("bXBT%d" % i, [128, 8, 512], BF16) for i in range(2)]; bXBT = [Buf("bXBT%d" % i) for i in range(2)]
        SA = [sb("bSA%d" % i, [128, 512], BF16) for i in range(2)]; bSA = [Buf("bSA%d" % i) for i in range(2)]
        HID = [sb("bHID%d" % i, [128, 4, 512], BF16) for i in range(2)]; bHID = [Buf("bHID%d" % i) for i in range(2)]
        YB = [sb("bYB%d" % i, [128, D], BF16) for i in range(3)]; bYB = [Buf("bYB%d" % i) for i in range(3)]
        X3L = [sb("bX3L%d" % i, [128, D], F32) for i in range(2)]; bX3L = [Buf("bX3L%d" % i) for i in range(2)]
        Y1 = [sb("bY1_%d" % i, [128, 2, D], BF16) for i in range(2)]; bY1 = [Buf("bY1_%d" % i) for i in range(2)]
        TK = [sb("bTK%d" % i, [128, 4], F32) for i in range(2)]; bTK = [Buf("bTK%d" % i) for i in range(2)]
        TKI = [sb("bTKI%d" % i, [128, 2], I32) for i in range(2)]; bTKI = [Buf("bTKI%d" % i) for i in range(2)]
        ST2 = sb("bST2", [128, 8], F32); bST2 = Buf("bST2")
        SC2 = sb("bSC2", [128, D], F32); bSC2 = Buf("bSC2")
        OUT = [sb("bOUT%d" % i, [128, D], F32) for i in range(2)]; bOUT = [Buf("bOUT%d" % i) for i in range(2)]
        QA = [ps("qA%d" % i, [128, 512], F32) for i in range(2)]; bQA = [Buf("qA%d" % i) for i in range(2)]
        QB = [ps("qB%d" % i, [128, 512], F32) for i in range(2)]; bQB = [Buf("qB%d" % i) for i in range(2)]
        QO = [ps("qO%d" % i, [128, 512], F32) for i in range(3)]; bQO = [Buf("qO%d" % i) for i in range(3)]
        QT_ = ps("qT", [128, 8, 128], BF16); bQT_ = Buf("qT")

        gate_ev = op("sp", lambda e: e.dma_start(out=GFN[:], in_=gfin_d), writes=[bGFN], dma=bGFN)
        for en in ("pe", "act", "dve", "pool"):
            sc._wait(en, gate_ev)
        op("sp", lambda e: e.dma_start(out=SM2[:], in_=sm2_d), writes=[bSM2], dma=bSM2)
        op("sp", lambda e: e.dma_start(out=IDN[:], in_=cb_d[:, 0:128]), writes=[bIDN], dma=bIDN)
        def T2(name, shape, dt=F32):
            return sb(name, shape, dt), Buf(name)
        LG, bLG = T2("rLG", [128, NCH, 20])
        GM, bGM = T2("rGM", [128, NCH])
        MG, bMG = T2("rMG", [128, 4, NCH])
        DG, bDG = T2("rDG", [128, 4, NCH])
        GS, bGS = T2("rGS", [128, NCH])
        EM, bEM = T2("rEM", [128, NCH, 16])
        EM2, bEM2 = T2("rEM2", [128, NCH, 16])
        MK1, bMK1 = T2("rMK1", [128, NCH, 16])
        MK2, bMK2 = T2("rMK2", [128, NCH, 16])
        M1, bM1 = T2("rM1", [128, NCH])
        M2, bM2 = T2("rM2", [128, NCH])
        PP, bPP = T2("rPP", [128, 2, NCH])
        ASGA, bASGA = T2("rASG", [128, NCH * 16], BF16)
        CSA, bCSA = T2("rCS", [128, NCH * 16])
        INA, bINA = T2("rINA", [128, NCH * 16])
        INB, bINB = T2("rINB", [128, NCH * 16])
        RKA, bRKA = T2("rRK", [128, NCH, 16])
        ONB, bONB = T2("rONB", [128, 128], BF16)
        STB, bSTB = T2("rSTB", [128, 128], BF16)
        op("sp", lambda e: e.dma_start(out=LG[:], in_=lg_d.rearrange("(n p) f -> p n f", p=128)), writes=[bLG], dma=bLG)
        op("sp", lambda e: e.dma_start(out=ONB[:], in_=cb_d[:, 256:384]), writes=[bONB], dma=bONB)
        op("sp", lambda e: e.dma_start(out=STB[:], in_=cb_d[:, 128:256]), writes=[bSTB], dma=bSTB)
        op("dve", lambda e: e.tensor_tensor(out=STB[:], in0=STB[:], in1=IDN[:], op=ALU.subtract), reads=[bSTB, bIDN], writes=[bSTB])
        op("dve", lambda e: e.reduce_max(out=GM[:], in_=LG[:, :, 0:4], axis=AX.X), reads=[bLG], writes=[bGM])
        for g in range(4):
            op("dve", lambda e, g=g: e.tensor_tensor(out=MG[:, g, :], in0=LG[:, :, g], in1=GM[:], op=ALU.is_ge),
               reads=[bLG, bGM], writes=[bMG])
            op("dve", lambda e, g=g: e.tensor_tensor(out=DG[:, g, :], in0=LG[:, :, g], in1=GM[:], op=ALU.subtract),
               reads=[bLG, bGM], writes=[bDG])
        op("act", lambda e: e.activation(out=DG[:], in_=DG[:], func=AF.Exp), reads=[bDG], writes=[bDG])
        op("dve", lambda e: e.tensor_tensor(out=GS[:], in0=DG[:, 0, :], in1=DG[:, 1, :], op=ALU.add), reads=[bDG], writes=[bGS])
        op("dve", lambda e: e.tensor_tensor(out=GS[:], in0=GS[:], in1=DG[:, 2, :], op=ALU.add), reads=[bDG, bGS], writes=[bGS])
        op("dve", lambda e: e.tensor_tensor(out=GS[:], in0=GS[:], in1=DG[:, 3, :], op=ALU.add), reads=[bDG, bGS], writes=[bGS])
        op("dve", lambda e: e.reciprocal(out=GS[:], in_=GS[:]), reads=[bGS], writes=[bGS])
        op("dve", lambda e: e.tensor_scalar(out=DG[:], in0=MG[:], scalar1=-1.0, scalar2=1e9, op0=ALU.add, op1=ALU.mult),
           reads=[bMG], writes=[bDG])
        for k in range(16):
            op("dve", lambda e, k=k: e.tensor_tensor(out=EM[:, :, k], in0=LG[:, :, 4 + k], in1=DG[:, k // 4, :], op=ALU.add),
               reads=[bLG, bDG], writes=[bEM])
        op("dve", lambda e: e.reduce_max(out=M1[:], in_=EM[:], axis=AX.X), reads=[bEM], writes=[bM1])
        for k in range(16):
            op("dve", lambda e, k=k: e.tensor_tensor(out=MK1[:, :, k], in0=EM[:, :, k], in1=M1[:], op=ALU.is_ge),
               reads=[bEM, bM1], writes=[bMK1])
        op("dve", lambda e: e.scalar_tensor_tensor(out=EM2[:].rearrange("p n k -> p (n k)"), in0=MK1[:].rearrange("p n k -> p (n k)"),
                                                   scalar=-1e9, in1=EM[:].rearrange("p n k -> p (n k)"), op0=ALU.mult, op1=ALU.add),
           reads=[bMK1, bEM], writes=[bEM2])
        op("dve", lambda e: e.reduce_max(out=M2[:], in_=EM2[:], axis=AX.X), reads=[bEM2], writes=[bM2])
        for k in range(16):
            op("dve", lambda e, k=k: e.tensor_tensor(out=MK2[:, :, k], in0=EM2[:, :, k], in1=M2[:], op=ALU.is_ge),
               reads=[bEM2, bM2], writes=[bMK2])
        op("dve", lambda e: e.tensor_tensor(out=PP[:, 0, :], in0=M2[:], in1=M1[:], op=ALU.subtract), reads=[bM1, bM2], writes=[bPP])
        op("act", lambda e: e.activation(out=PP[:, 0, :], in_=PP[:, 0, :], func=AF.Exp), reads=[bPP], writes=[bPP])
        op("dve", lambda e: e.tensor_scalar_add(out=PP[:, 0, :], in0=PP[:, 0, :], scalar1=1.0), reads=[bPP], writes=[bPP])
        op("dve", lambda e: e.reciprocal(out=PP[:, 0, :], in_=PP[:, 0, :]), reads=[bPP], writes=[bPP])
        op("dve", lambda e: e.tensor_scalar(out=PP[:, 1, :], in0=PP[:, 0, :], scalar1=-1.0, scalar2=1.0, op0=ALU.mult, op1=ALU.add),
           reads=[bPP], writes=[bPP])
        for k in range(2):
            op("dve", lambda e, k=k: e.tensor_tensor(out=WAB[:, :, k], in0=PP[:, k, :], in1=GS[:], op=ALU.mult),
               reads=[bPP, bGS], writes=[bWAB])
        op("dve", lambda e: e.tensor_tensor(out=ASGA[:], in0=MK1[:].rearrange("p n k -> p (n k)"),
                                            in1=MK2[:].rearrange("p n k -> p (n k)"), op=ALU.add), reads=[bMK1, bMK2], writes=[bASGA])
        NH = (NCH * 16 + 511) // 512
        for hf in range(NH):
            w = min(512, NCH * 16 - hf * 512)
            op("pe", lambda e, hf=hf, w=w: e.matmul(out=QA[hf % 2][:, 0:w], lhsT=ONB[:], rhs=ASGA[:, hf * 512:hf * 512 + w],
                                                    start=True, stop=True), reads=[bONB, bASGA], writes=[bQA[hf % 2]])
            op("dve", lambda e, hf=hf, w=w: e.tensor_copy(out=CSA[:, hf * 512:hf * 512 + w], in_=QA[hf % 2][:, 0:w]),
               reads=[bQA[hf % 2]], writes=[bCSA])
            op("pe", lambda e, hf=hf, w=w: e.matmul(out=QB[hf % 2][:, 0:w], lhsT=STB[:], rhs=ASGA[:, hf * 512:hf * 512 + w],
                                                    start=True, stop=True), reads=[bSTB, bASGA], writes=[bQB[hf % 2]])
            op("dve", lambda e, hf=hf, w=w: e.tensor_copy(out=RKA[:].rearrange("p n k -> p (n k)")[:, hf * 512:hf * 512 + w],
                                                          in_=QB[hf % 2][:, 0:w]), reads=[bQB[hf % 2]], writes=[bRKA])
        op("dve", lambda e: e.tensor_copy(out=INA[:], in_=CSA[:]), reads=[bCSA], writes=[bINA])
        cur, bcur, nxt, bnxt = INA, bINA, INB, bINB
        sft = 1
        while sft < NCH:
            o = sft * 16
            op("dve", lambda e, cur=cur, nxt=nxt, o=o: e.tensor_tensor(out=nxt[:, o:], in0=cur[:, o:], in1=cur[:, 0:NCH * 16 - o],
                                                                      op=ALU.add), reads=[bcur], writes=[bnxt])
            op("dve", lambda e, cur=cur, nxt=nxt, o=o: e.tensor_copy(out=nxt[:, 0:o], in_=cur[:, 0:o]), reads=[bcur], writes=[bnxt])
            cur, bcur, nxt, bnxt = nxt, bnxt, cur, bcur
            sft *= 2
        INC, bINC = cur, bcur
        op("dve", lambda e: e.tensor_tensor(out=CSA[:], in0=INC[:], in1=CSA[:], op=ALU.subtract), reads=[bINC, bCSA], writes=[bCSA])
        op("dve", lambda e: e.tensor_tensor(out=RKA[:].rearrange("p n k -> p (n k)"), in0=RKA[:].rearrange("p n k -> p (n k)"),
                                            in1=CSA[:], op=ALU.add), reads=[bRKA, bCSA], writes=[bRKA])
        op("dve", lambda e: e.tensor_copy(out=CNT[:], in_=INC[:, (NCH - 1) * 16:NCH * 16]), reads=[bINC], writes=[bCNT])
        op("dve", lambda e: e.tensor_scalar(out=CNT[:], in0=CNT[:], scalar1=float(BS - 1), scalar2=1.0 / BS,
                                            op0=ALU.add, op1=ALU.mult), reads=[bCNT], writes=[bCNT])
        op("dve", lambda e: e.tensor_copy(out=NBi[:], in_=CNT[:]), reads=[bCNT], writes=[bNBi])
        op("dve", lambda e: e.tensor_copy(out=NBf[:], in_=NBi[:]), reads=[bNBi], writes=[bNB])
        op("dve", lambda e: e.tensor_tensor(out=MSK[:, 0:16], in0=NBf[:], in1=CNT[:], op=ALU.is_gt),
           reads=[bNB, bCNT], writes=[bMSK])
        op("dve", lambda e: e.tensor_tensor(out=NBf[:], in0=NBf[:], in1=MSK[:, 0:16], op=ALU.subtract),
           reads=[bNB, bMSK], writes=[bNB])
        op("dve", lambda e: e.tensor_copy(out=CUM[:, 0:1], in_=NBf[:, 0:1]), reads=[bNB], writes=[bCUM])
        for e_ in range(1, NE):
            op("dve", lambda e, e_=e_: e.tensor_tensor(out=CUM[:, e_:e_ + 1], in0=CUM[:, e_ - 1:e_], in1=NBf[:, e_:e_ + 1],
                                                       op=ALU.add), reads=[bCUM, bNB], writes=[bCUM])
        for j in range(NBLK):
            op("dve", lambda e, j=j: e.tensor_single_scalar(out=MSK[:, 0:16], in_=CUM[:], scalar=float(j), op=ALU.is_le),
               reads=[bCUM], writes=[bMSK])
            op("dve", lambda e, j=j: e.reduce_sum(out=EJ[:, j:j + 1], in_=MSK[:, 0:16], axis=AX.X), reads=[bMSK], writes=[bEJ])
        op("dve", lambda e: e.tensor_scalar_min(out=EJ[:], in0=EJ[:], scalar1=float(NE - 1)), reads=[bEJ], writes=[bEJ])
        op("dve", lambda e: e.tensor_scalar(out=EW[:, 0, :], in0=EJ[:], scalar1=128.0, scalar2=SM2[:, 16:17],
                                            op0=ALU.mult, op1=ALU.add), reads=[bEJ, bSM2], writes=[bEW])
        op("dve", lambda e: e.tensor_scalar(out=EW[:, 1, :], in0=EJ[:], scalar1=float(FF), scalar2=SM2[:, 16:17],
                                            op0=ALU.mult, op1=ALU.add), reads=[bEJ, bSM2], writes=[bEW])
        for s_ in range(4):
            op("dve", lambda e, s_=s_: e.tensor_scalar_add(out=IXF[:, :, 12 + s_], in0=EW[:, 1, :], scalar1=float(s_ * 128)),
               reads=[bEW], writes=[bIXF])
        for kc in range(8):
            op("dve", lambda e, kc=kc: e.tensor_scalar_add(out=IXF[:, :, 4 + kc], in0=EW[:, 0, :], scalar1=0.0),
               reads=[bEW], writes=[bIXF])
        op("dve", lambda e: e.tensor_copy(out=IXI[:, :, 4:16], in_=IXF[:, :, 4:16]), reads=[bIXF], writes=[bIXI])
        op("dve", lambda e: e.tensor_tensor(out=PST[:], in0=CUM[:], in1=NBf[:], op=ALU.subtract), reads=[bCUM, bNB], writes=[bPST])
        op("dve", lambda e: e.tensor_scalar(out=PST[:], in0=PST[:], scalar1=float(BS), scalar2=None, op0=ALU.mult),
           reads=[bPST], writes=[bPST])
        for e_ in range(NE):
            op("dve", lambda e, e_=e_: e.tensor_scalar(out=RKA[:, :, e_], in0=RKA[:, :, e_], scalar1=PST[:, e_:e_ + 1], scalar2=None,
                                                       op0=ALU.add), reads=[bRKA, bPST], writes=[bRKA])
        op("dve", lambda e: e.tensor_tensor(out=EM[:].rearrange("p n k -> p (n k)"), in0=MK1[:].rearrange("p n k -> p (n k)"),
                                            in1=RKA[:].rearrange("p n k -> p (n k)"), op=ALU.mult), reads=[bMK1, bRKA], writes=[bEM])
        op("dve", lambda e: e.reduce_sum(out=M1[:], in_=EM[:], axis=AX.X), reads=[bEM], writes=[bM1])
        op("dve", lambda e: e.tensor_tensor(out=EM2[:].rearrange("p n k -> p (n k)"), in0=MK2[:].rearrange("p n k -> p (n k)"),
                                            in1=RKA[:].rearrange("p n k -> p (n k)"), op=ALU.mult), reads=[bMK2, bRKA], writes=[bEM2])
        op("dve", lambda e: e.reduce_sum(out=M2[:], in_=EM2[:], axis=AX.X), reads=[bEM2], writes=[bM2])
        op("dve", lambda e: e.tensor_copy(out=DPI[:, :, 0], in_=M1[:]), reads=[bM1], writes=[bDPI])
        op("dve", lambda e: e.tensor_copy(out=DPI[:, :, 1], in_=M2[:]), reads=[bM2], writes=[bDPI])
        for t in range(NCH):
            i = t % 2
            op("sp", lambda e, t=t, i=i: e.dma_start(out=H3L[i][:], in_=h3_d[t * 128:(t + 1) * 128, :]), writes=[bH3L[i]], dma=bH3L[i])
            for k in range(2):
                op("pool", lambda e, t=t, k=k, i=i: e.indirect_dma_start(
                    out=xs_d[:, :], out_offset=bass.IndirectOffsetOnAxis(ap=DPI[:, t, k:k + 1], axis=0),
                    in_=H3L[i][:], in_offset=None), reads=[bH3L[i], bDPI], dma=bH3L[i])
        for key, cnt_ in list(sc.dcnt.items()):
            sc._wait("sp", (key, 16 * cnt_))

        w1f = wb1_d.rearrange("(l a) c -> l (a c)", a=4)
        w3f = wb3_d.rearrange("(l a) c -> l (a c)", a=4)
        w2f = wb2_d.rearrange("(l a) c -> l (a c)", a=4)

        def gat(dst, src, j, col, buf):
            op("pool", lambda e: e.indirect_dma_start(out=dst, out_offset=None, in_=src,
                                                      in_offset=bass.IndirectOffsetOnAxis(ap=IXI[:, j, col:col + 1], axis=0)),
               reads=[bIXI], writes=[buf], dma=buf)

        def load_block(j):
            i = j % 2
            op("sp", lambda e: e.dma_start(out=XB[i][:], in_=xs_d[j * BS:(j + 1) * BS, :].rearrange("(s p) d -> p s d", p=128)),
               writes=[bXB[i]], dma=bXB[i])
            gat(W1[i][:].rearrange("p k f -> p (k f)"), w1f, j, 4, bW[i])
            gat(W3[i][:].rearrange("p k f -> p (k f)"), w3f, j, 4, bW[i])
            gat(W2[i][:].rearrange("p k f -> p (k f)"), w2f, j, 4, bW[i])

        ab_i = 0
        o_i = 0
        y_i = 0
        load_block(0)
        for j in range(NBLK):
            i = j % 2
            if j + 1 < NBLK:
                load_block(j + 1)
            for s_ in range(4):
                for kc in range(8):
                    op("pe", lambda e, s_=s_, kc=kc, i=i: e.transpose(
                        out=QT_[:, kc, :], in_=XB[i][:, s_, :].rearrange("p (a k) -> p k a", k=8)[:, kc, :],
                        identity=IDN[:]), reads=[bXB[i], bIDN], writes=[bQT_])
                if s_ % 2 == 0:
                    op("dve", lambda e, s_=s_, i=i: e.tensor_copy(out=XBT[i][:, :, s_ * 128:(s_ + 1) * 128], in_=QT_[:]),
                       reads=[bQT_], writes=[bXBT[i]])
                else:
                    op("act", lambda e, s_=s_, i=i: e.activation(out=XBT[i][:, :, s_ * 128:(s_ + 1) * 128], in_=QT_[:], func=AF.Copy),
                       reads=[bQT_], writes=[bXBT[i]])
            for f in range(4):
                ai = ab_i % 2
                ab_i += 1
                for kc in range(8):
                    op("pe", lambda e, kc=kc, f=f, ai=ai, i=i: e.matmul(
                        out=QA[ai][:, :], lhsT=W1[i][:, kc, :].rearrange("p (a r) -> p r a", r=4)[:, f, :], rhs=XBT[i][:, kc, :],
                        start=(kc == 0), stop=(kc == 7)), reads=[bW[i], bXBT[i]], writes=[bQA[ai]])
                for kc in range(8):
                    op("pe", lambda e, kc=kc, f=f, ai=ai, i=i: e.matmul(
                        out=QB[ai][:, :], lhsT=W3[i][:, kc, :].rearrange("p (a r) -> p r a", r=4)[:, f, :], rhs=XBT[i][:, kc, :],
                        start=(kc == 0), stop=(kc == 7)), reads=[bW[i], bXBT[i]], writes=[bQB[ai]])
                op("act", lambda e, ai=ai: e.activation(out=SA[ai][:], in_=QA[ai][:, :], func=AF.Silu),
                   reads=[bQA[ai]], writes=[bSA[ai]])
                op("dve", lambda e, ai=ai, hb=j % 2, f=f: e.tensor_tensor(out=HID[hb][:, f, :], in0=QB[ai][:, :], in1=SA[ai][:], op=ALU.mult),
                   reads=[bQB[ai], bSA[ai]], writes=[bHID[j % 2]])
            for s_ in range(4):
                yi = y_i % 3
                y_i += 1
                for half in range(2):
                    oi = o_i % 3
                    o_i += 1
                    for f in range(4):
                        op("pe", lambda e, f=f, s_=s_, half=half, oi=oi, i=i, hb=j % 2: e.matmul(
                            out=QO[oi][:, :], lhsT=HID[hb][:, f, s_ * 128:(s_ + 1) * 128],
                            rhs=W2[i][:, f, half * 512:(half + 1) * 512], start=(f == 0), stop=(f == 3)),
                           reads=[bHID[j % 2], bW[i]], writes=[bQO[oi]])
                    if half == 0:
                        op("act", lambda e, oi=oi, yi=yi: e.activation(out=YB[yi][:, 0:512], in_=QO[oi][:, :], func=AF.Copy),
                           reads=[bQO[oi]], writes=[bYB[yi]])
                    else:
                        op("dve", lambda e, oi=oi, yi=yi: e.tensor_copy(out=YB[yi][:, 512:1024], in_=QO[oi][:, :]),
                           reads=[bQO[oi]], writes=[bYB[yi]])
                op("sp", lambda e, yi=yi, j=j, s_=s_: e.dma_start(out=ys_d[j * BS + s_ * 128:j * BS + (s_ + 1) * 128, :],
                                                                 in_=YB[yi][:]), reads=[bYB[yi]], dma=bYB[yi])
        for key, cnt_ in list(sc.dcnt.items()):
            sc._wait("pool", (key, 16 * cnt_))
        for t in range(NCH):
            i = t % 2
            op("sp", lambda e, t=t, i=i: e.dma_start(out=X3L[i][:], in_=out_d[t * 128:(t + 1) * 128, :]), writes=[bX3L[i]], dma=bX3L[i])
            for k in range(2):
                op("pool", lambda e, i=i, k=k, t=t: e.indirect_dma_start(
                    out=Y1[i][:, k, :], out_offset=None, in_=ys_d[:, :],
                    in_offset=bass.IndirectOffsetOnAxis(ap=DPI[:, t, k:k + 1], axis=0)), reads=[bDPI], writes=[bY1[i]], dma=bY1[i])
            op("dve", lambda e, i=i, t=t: e.scalar_tensor_tensor(out=X3L[i][:], in0=Y1[i][:, 0, :], scalar=WAB[:, t, 0:1], in1=X3L[i][:],
                                                                op0=ALU.mult, op1=ALU.add), reads=[bY1[i], bWAB, bX3L[i]], writes=[bX3L[i]])
            op("dve", lambda e, i=i, t=t: e.scalar_tensor_tensor(out=X3L[i][:], in0=Y1[i][:, 1, :], scalar=WAB[:, t, 1:2], in1=X3L[i][:],
                                                                op0=ALU.mult, op1=ALU.add), reads=[bY1[i], bWAB, bX3L[i]], writes=[bX3L[i]])
            op("act", lambda e, i=i: e.activation(out=SC2[:], in_=X3L[i][:], func=AF.Square, accum_out=ST2[:, 0:1]),
               reads=[bX3L[i]], writes=[bSC2, bST2])
            op("dve", lambda e: e.tensor_scalar(out=ST2[:, 0:1], in0=ST2[:, 0:1], scalar1=1.0 / D, scalar2=EPS,
                                                op0=ALU.mult, op1=ALU.add), reads=[bST2], writes=[bST2])
            op("act", lambda e: e.activation(out=ST2[:, 0:1], in_=ST2[:, 0:1], func=AF.Ln), reads=[bST2], writes=[bST2])
            op("act", lambda e: e.activation(out=ST2[:, 0:1], in_=ST2[:, 0:1], func=AF.Exp, scale=-0.5),
               reads=[bST2], writes=[bST2])
            op("dve", lambda e, i=i: e.scalar_tensor_tensor(out=OUT[i][:], in0=X3L[i][:], scalar=ST2[:, 0:1], in1=GFN[:],
                                                           op0=ALU.mult, op1=ALU.mult), reads=[bX3L[i], bST2, bGFN], writes=[bOUT[i]])
            op("sp", lambda e, t=t, i=i: e.dma_start(out=out_d[t * 128:(t + 1) * 128, :], in_=OUT[i][:]), reads=[bOUT[i]], dma=bOUT[i])
        sc.finish("sp")
        sc.emit()
    semstack.__exit__(None, None, None)
    return nc


def _consts():
    ident = np.eye(128, dtype=np.float32)
    s = np.arange(128)[:, None]
    l = np.arange(128)[None, :]
    tri = (s <= l).astype(np.float32)
    ones = np.ones((128, 128), np.float32)
    cbf = np.concatenate([ident, tri, ones], axis=1).astype(ml_dtypes.bfloat16)
    inv_freq = (500000.0 ** (-np.arange(0, 16, 2, dtype=np.float32) / 16)).astype(np.float32)
    invE = np.tile(np.tile(inv_freq, 8)[None, :], (128, 1)).astype(np.float32)
    cf32 = np.concatenate([tri, ones, invE], axis=1).astype(np.float32)
    am = np.zeros((128, NJ * 128), np.float32)
    for j in range(NJ):
        dist = 128 * j + l - s
        m = ((dist >= 0) & (dist <= 128)).astype(np.float32)
        m += ((dist >= 0) & (dist % 4 == 0) & (dist <= 512))
        m += ((dist >= 0) & (dist % 16 == 0) & (dist <= 2048))
        am[:, j * 128:(j + 1) * 128] = np.where(m > 0, 8.0 * np.log(np.maximum(m, 1.0)), -240000.0)
    return cbf, cf32, am.astype(ml_dtypes.bfloat16)


def make_in_maps(inp, S, ncores):
    cbf, cf32, am = _consts()
    f = lambda a: np.ascontiguousarray(np.asarray(a, dtype=np.float32))
    gk = np.zeros((128, 32), np.float32)
    gk[:, 0:8] = f(inp["g_mix"])[0].reshape(8, 128).T
    gk[:, 8:16] = f(inp["g_cross"])[0].reshape(8, 128).T
    gk[:, 16:24] = f(inp["g_mem"])[0].reshape(8, 128).T
    sm = np.zeros((128, 64), np.float32)
    sm[:, 0:4] = f(inp["b_i"])[0][None, :]
    sm[:, 4:8] = f(inp["b_f"])[0][None, :]
    cw = f(inp["conv_w"])[0]
    sm[:, 8:24] = cw.reshape(4, 4, 128).transpose(2, 1, 0).reshape(128, 16)
    sm[:, 24:28] = f(inp["conv_b"])[0].reshape(4, 128).T
    sm[:, 28:32] = f(inp["g_mhn"])[0].reshape(4, 128).T
    sm[:, 32:36] = f(inp["skip_m"])[0].reshape(4, 128).T
    sm[:, 36:40] = f(inp["b_router_g"])[0][None, :]
    sm[:, 40:56] = f(inp["b_router_e"])[0][None, :]
    gff = np.ascontiguousarray(np.tile(f(inp["g_ffn"])[0][None, :], (128, 1)))
    gfin = np.ascontiguousarray(np.tile(f(inp["g_final"])[None, :], (128, 1)))
    w_r = np.ascontiguousarray(np.concatenate([f(inp["w_router_g"])[0], f(inp["w_router_e"])[0]], axis=1))
    sm2 = np.zeros((128, 48), np.float32)
    sm2[:, 32:48] = np.arange(16, dtype=np.float32)[None, :]
    sm2[:, 0:16] = (np.arange(16, dtype=np.float32) * S)[None, :]
    sm2[:, 16] = np.arange(128, dtype=np.float32)
    shared = {
        "sm2": sm2,
        "w_in": f(inp["w_in"])[0], "w_out": f(inp["w_out"])[0], "w_q_m": f(inp["w_q_m"])[0], "w_k_m": f(inp["w_k_m"])[0],
        "w_q_x": f(inp["w_q_x"])[0], "w_kv_x": f(inp["w_kv_x"])[0], "w_o_x": f(inp["w_o_x"])[0], "w_r": w_r,
        "w1": f(inp["w1"])[0], "w3": f(inp["w3"])[0], "w2": f(inp["w2"])[0],
        "gk": gk, "sm": sm, "gff": gff, "gfin": gfin, "cbf": cbf, "cf32": cf32, "amask": am,
    }
    x = np.asarray(inp["x"]); mem = np.asarray(inp["mem"]); pos = np.asarray(inp["positions"])
    maps = []
    for b in range(ncores):
        m = dict(shared)
        m["x"] = np.ascontiguousarray(x[b, :S], dtype=np.float32)
        m["mem"] = np.ascontiguousarray(mem[b], dtype=np.float32)
        m["pos"] = np.ascontiguousarray(pos[b, :S].astype(np.int32).reshape(S // 128, 128).T)
        maps.append(m)
    return maps


def kernel(**inputs):
    S = 8192
    nc = build(S)
    maps = make_in_maps(inputs, S, 8)
    res = run_bass_kernel_spmd(nc, maps, core_ids=list(range(8)))
    return np.stack([np.asarray(r["out"], dtype=np.float32) for r in res.results], axis=0)
```

```python
from contextlib import ExitStack
import math
import numpy as np
import ml_dtypes
import concourse.bass as bass
import concourse.mybir as mybir
from concourse.bass_utils import run_bass_kernel_spmd

F32 = mybir.dt.float32
BF16 = mybir.dt.bfloat16
I32 = mybir.dt.int32
AF = mybir.ActivationFunctionType
ALU = mybir.AluOpType
AX = mybir.AxisListType

D = 1024
NE = 16
FF = 512
MEM = 256
EPS = 1e-6
NJ = 17
SEG = 20000


class Buf:
    def __init__(self, name, ro=False):
        self.name = name
        self.w = None
        self.r = []
        self.ro = ro


class Sched:
    def __init__(self, nc, stack, tag):
        self.nc, self.stack, self.tag = nc, stack, tag
        self.gate = None
        self.sems = {}
        self.prog = {e: [] for e in ("pe", "act", "dve", "pool", "sp")}
        self.cnt = {e: 0 for e in self.prog}
        self.waited = {e: {} for e in self.prog}
        self.dcnt = {}
        self.deferq = []
        self.defer = False

    def flush(self, n=None):
        k = len(self.deferq) if n is None else min(n, len(self.deferq))
        items, self.deferq = self.deferq[:k], self.deferq[k:]
        for it in items:
            self._op(*it)

    def sem(self, key):
        if key not in self.sems:
            self.sems[key] = self.stack.enter_context(
                self.nc.semaphore("%s_s%d" % (self.tag, len(self.sems))))
        return self.sems[key]

    def _wait(self, eng, ev):
        key, v = ev
        if key[0] == "eng":
            if key[1] == "pe" and eng == "pe":
                return
            k2 = ("eng", key[1])
            g = key[2] * SEG + v
            if self.waited[eng].get(k2, 0) >= g:
                return
            self.waited[eng][k2] = g
        else:
            if self.waited[eng].get(key, 0) >= v:
                return
            self.waited[eng][key] = v
        self.sem(key)
        self.prog[eng].append(("wait", key, v))

    def op(self, eng, fn, reads=(), writes=(), dma=None):
        if self.defer:
            self.deferq.append((eng, fn, tuple(reads), tuple(writes), dma))
            return None
        return self._op(eng, fn, reads, writes, dma)

    def _op(self, eng, fn, reads=(), writes=(), dma=None):
        for b in reads:
            if b.w is not None:
                self._wait(eng, b.w)
        for b in writes:
            if b.w is not None:
                self._wait(eng, b.w)
            for ev in b.r:
                self._wait(eng, ev)
        if dma is not None:
            key = ("dma", id(dma))
            self.dcnt[key] = self.dcnt.get(key, 0) + 1
            ev = (key, 16 * self.dcnt[key])
            self.sem(key)
            self.prog[eng].append(("dma", fn, key))
        else:
            n = self.cnt[eng]
            self.cnt[eng] += 1
            key = ("eng", eng, n // SEG)
            ev = (key, n % SEG + 1)
            self.sem(key)
            self.prog[eng].append(("op", fn, key))
        for b in reads:
            if not b.ro:
                b.r.append(ev)
                if len(b.r) > 64:
                    b.r = b.r[-48:]
        for b in writes:
            b.w = ev
            b.r = []
        return ev

    def finish(self, eng="sp"):
        for key, c in list(self.dcnt.items()):
            self._wait(eng, (key, 16 * c))
        for e in ("pe", "act", "dve", "pool"):
            n = self.cnt[e]
            if n > 0 and e != eng:
                self._wait(eng, (("eng", e, (n - 1) // SEG), (n - 1) % SEG + 1))

    def emit(self):
        nc = self.nc
        sems = self.sems
        prog = self.prog

        def run(name, e):
            for it in prog[name]:
                if it[0] == "wait":
                    e.wait_ge(sems[it[1]], it[2])
                elif it[0] == "op":
                    it[1](e).then_inc(sems[it[2]], 1)
                else:
                    it[1](e).then_inc(sems[it[2]], 16)

        with nc.Block() as block:
            @block.sync
            def _(e):
                run("sp", e)

            @block.scalar
            def _(e):
                run("act", e)

            @block.vector
            def _(e):
                run("dve", e)

            @block.gpsimd
            def _(e):
                run("pool", e)

            @block.tensor
            def _(e):
                run("pe", e)


def build(S, debug=False):
    NCH = S // 128
    SPAN = min(2048, S)
    NSP = S // SPAN
    nc = bass.Bass("TRN2", target_bir_lowering=False)

    def din(name, shape, dt=F32):
        return nc.dram_tensor(name, list(shape), dt, kind="ExternalInput").ap()

    x_d = din("x", [S, D])
    mem_d = din("mem", [MEM, D])
    pos_d = din("pos", [128, NCH], I32)
    w_in_d = din("w_in", [D, 3080])
    w_out_d = din("w_out", [D, D])
    wqm_d = din("w_q_m", [4, 128, 128])
    wkm_d = din("w_k_m", [4, 128, 128])
    wqx_d = din("w_q_x", [D, 256])
    wkv_d = din("w_kv_x", [D, 512])
    wox_d = din("w_o_x", [256, D])
    wr_d = din("w_r", [D, 20])
    w1_d = din("w1", [NE, D, FF])
    w3_d = din("w3", [NE, D, FF])
    w2_d = din("w2", [NE, FF, D])
    gk_d = din("gk", [128, 32])
    sm_d = din("sm", [128, 64])
    gff_d = din("gff", [128, D])
    gfin_d = din("gfin", [128, D])
    cb_d = din("cbf", [128, 128 * 3], BF16)
    cf_d = din("cf32", [128, 128 * 2 + 64])
    am_d = din("amask", [128, NJ * 128], BF16)
    out_d = nc.dram_tensor("out", [S, D], F32, kind="ExternalOutput").ap()
    h3T_d = nc.dram_tensor("h3T_scr", [8, 128, S], BF16, kind="Internal").ap()
    BS = 512
    NBLK = (2 * S) // BS + NE
    CAP = S
    NROWS = NBLK * BS
    h3_d = nc.dram_tensor("h3_scr", [S, D], BF16, kind="Internal").ap()
    wb1_d = nc.dram_tensor("wb1_scr", [8192, 1024], BF16, kind="Internal").ap()
    wb3_d = nc.dram_tensor("wb3_scr", [8192, 1024], BF16, kind="Internal").ap()
    wb2_d = nc.dram_tensor("wb2_scr", [8192, 1024], BF16, kind="Internal").ap()
    w1v = w1_d.rearrange("e (k two) f -> (e k) (two f)", two=2)
    w3v = w3_d.rearrange("e (k two) f -> (e k) (two f)", two=2)
    w2v = w2_d.rearrange("e k f -> (e k) f")
    lg_d = nc.dram_tensor("lg_scr", [S, 20], F32, kind="Internal").ap()
    cnt_d = nc.dram_tensor("cnt_scr", [128, NE], F32, kind="Internal").ap()
    xs_d = nc.dram_tensor("xs_scr", [NROWS, D], BF16, kind="Internal").ap()
    ys_d = nc.dram_tensor("ys_scr", [NROWS, D], BF16, kind="Internal").ap()
    sm2_d = din("sm2", [128, 48])
    if debug:
        dbg2_d = nc.dram_tensor("dbg2", [S, D], F32, kind="ExternalOutput").ap()
        dbg3_d = nc.dram_tensor("dbg3", [S, D], F32, kind="ExternalOutput").ap()

    semstack = ExitStack()
    semstack.__enter__()
    with ExitStack() as st:
        sc = Sched(nc, semstack, "A")

        def sb(name, shape, dt):
            return st.enter_context(nc.sbuf_tensor(name, list(shape), dt))

        def ps(name, shape, dt):
            return st.enter_context(nc.psum_tensor(name, list(shape), dt))

        WIN = sb("WIN", [128, 8, 3080], BF16); bWIN = Buf("WIN", ro=True)
        WOA = sb("WOA", [128, 4, D], BF16); bWOA = Buf("WOA", ro=True)
        WOM = sb("WOM", [128, 4, D], BF16); bWOM = Buf("WOM", ro=True)
        WQX = sb("WQX", [128, 8, 256], BF16); bWQX = Buf("WQX", ro=True)
        WKV = sb("WKV", [128, 8, 512], BF16); bWKV = Buf("WKV", ro=True)
        WOX = sb("WOX", [128, 2, D], BF16); bWOX = Buf("WOX", ro=True)
        WQM = sb("WQM", [128, 4, 128], BF16); bWQM = Buf("WQM", ro=True)
        WKM = sb("WKM", [128, 4, 128], BF16); bWKM = Buf("WKM", ro=True)
        WRH = sb("WRH", [128, 8, 20], BF16); bWRH = Buf("WRH", ro=True)
        WRL = sb("WRL", [128, 8, 20], BF16); bWRL = Buf("WRL", ro=True)
        WR32 = sb("WR32", [128, 8, 20], F32); bWR32 = Buf("WR32")
        WRT = sb("WRT", [128, 8, 20], F32); bWRT = Buf("WRT")
        GK = sb("GK", [128, 32], F32); bGK = Buf("GK", ro=True)
        SM = sb("SM", [128, 64], F32); bSM = Buf("SM", ro=True)
        GFF = sb("GFF", [128, D], F32); bGFF = Buf("GFF", ro=True)
        CB = sb("CB", [128, 384], BF16); bCB = Buf("CB", ro=True)
        CF = sb("CF", [128, 320], F32); bCF = Buf("CF", ro=True)
        AM = sb("AM", [128, NJ * 128], BF16); bAM = Buf("AM", ro=True)
        POSI = sb("POSI", [128, NCH], I32); bPOSI = Buf("POSI", ro=True)
        POSF = sb("POSF", [128, NCH], F32); bPOSF = Buf("POSF", ro=True)
        STG = [sb("STG%d" % i, [128, 1032], F32) for i in range(2)]
        bSTG = [Buf("STG%d" % i) for i in range(2)]
        ident = CB[:, 0:128]; tri_b = CB[:, 128:256]
        tri_f = CF[:, 0:128]; ones_f = CF[:, 128:256]; invE = CF[:, 256:320]
        KTR = sb("KTR", [128, 4, NJ * 128], BF16); bKTR = [Buf("KTR%d" % i) for i in range(NJ)]
        VAR = sb("VAR", [128, NJ, 8, 65], BF16); bVAR = [Buf("VAR%d" % i) for i in range(NJ)]
        XT = [sb("XT%d" % i, [128, D], F32) for i in range(3)]; bXT = [Buf("XT%d" % i) for i in range(3)]
        XN = sb("XN", [128, D], BF16); bXN = Buf("XN")
        HT = sb("HT", [128, 8, 128], BF16); bHT = Buf("HT")
        SCR = sb("SCR", [128, 128], F32); bSCR = Buf("SCR")
        ST = sb("STAT", [128, 64], F32); bST = Buf("STAT")
        ROT = sb("ROT", [128, 6, 64], F32); bROT = Buf("ROT")
        TRG = sb("TRG", [128, 2, 64], F32); bTRG = Buf("TRG")
        ROTI = sb("ROTI", [128, 2, 64], I32); bROTI = Buf("ROTI")
        QR = sb("QR", [128, 512], BF16); bQR = Buf("QR")
        KR = sb("KR", [128, 512], BF16); bKR = Buf("KR")
        QT = sb("QT", [128, 4, 2, 128], BF16); bQT = Buf("QT")
        PT = [sb("PT%d" % i, [128, 512], BF16) for i in range(2)]; bPT = [Buf("PT%d" % i) for i in range(2)]
        RDS = sb("RDS", [128, 8, 1], F32); bRD = Buf("RD")
        RDX = sb("RDX", [128, 4, 1], F32); bRDX = Buf("RDX")
        YAT = sb("YAT", [128, 512], BF16); bYAT = Buf("YAT")
        YA = sb("YA", [128, 4, 128], BF16); bYA = Buf("YA")
        MUB = sb("MUB", [128, 4, 131], F32); bMUB = Buf("MUB")
        CPRE = sb("CPRE", [128, 4, 128], F32); bCPRE = Buf("CPRE")
        CT = sb("CT", [128, 4, 128], BF16); bCT = Buf("CT")
        MQT = sb("MQT", [128, 4, 128], BF16); bMQT = Buf("MQT")
        MKT = sb("MKT", [128, 4, 128], BF16); bMKT = Buf("MKT")
        KG = sb("KG", [128, 4, 128], BF16); bKG = Buf("KG")
        VAM = sb("VAM", [128, 4, 129], BF16); bVAM = Buf("VAM")
        GT = sb("GT", [128, 64], F32); bGT = Buf("GT")
        STL4 = sb("STL4", [128, 4, 128], BF16); bSTLh = [Buf("STL%d" % i) for i in range(4)]
        bCPh = [Buf("CPh%d" % i) for i in range(4)]; bCFSh = [Buf("CFSh%d" % i) for i in range(4)]
        bYMh = [Buf("YMh%d" % i) for i in range(4)]; bSSh = [Buf("SSh%d" % i) for i in range(4)]; bEP = Buf("EP")
        CFS = sb("CFS", [128, 4, 129], F32); bCFS = Buf("CFS")
        CBS = sb("CBS", [128, 4, 129], BF16); bCBS = Buf("CBS")
        HN = sb("HN", [128, 4, 128], BF16); bHN = Buf("HN")
        SGB = sb("SGB", [128, 512], BF16); bSG = Buf("SG")
        YM = sb("YM", [128, 4, 128], BF16); bYM = Buf("YM")
        X2 = sb("X2", [128, D], F32); bX2 = Buf("X2")
        X3 = [sb("X3_0", [128, D], F32)] * 2; bX3 = [Buf("X3_0")] * 2
        H2T = sb("H2T", [128, 8, 128], BF16); bH2T = Buf("H2T")
        QXT = sb("QXT", [64, 4, 128], BF16); bQXT = Buf("QXT")
        KXT = sb("KXT", [64, 4, MEM], BF16); bKXT = Buf("KXT", ro=True)
        VXA = sb("VXA", [128, 2, 4, 65], BF16); bVXA = Buf("VXA", ro=True)
        PXT = sb("PXT", [128, 8, 128], BF16); bPXT = Buf("PXT")
        OXT = sb("OXT", [128, 2, 128], BF16); bOXT = Buf("OXT")
        OXK = sb("OXK", [128, 256], BF16); bOXK = Buf("OXK")
        XN3 = X2; bXN3 = bX2
        HI = XN; bHI = bXN
        LO = sb("LO", [128, D], BF16); bLO = Buf("LO")
        H3T = [sb("H3T0", [128, 8, 128], BF16)] * 2; bH3T = [Buf("H3T0")] * 2
        L3T = H2T; bL3T = bH2T
        RT = sb("RT", [128, 160], F32); bRT = Buf("RT")
        SM2 = sb("SM2", [128, 48], F32); bSM2 = Buf("SM2", ro=True)
        STRI = sb("STRI", [128, 128], BF16); bSTRI = Buf("STRI", ro=True)
        ASG = sb("ASG", [128, NE], BF16); bASG = Buf("ASG")
        BASE = sb("BASE", [128, NE], F32); bBASE = Buf("BASE")
        RK = sb("RK", [128, 48], F32); bRK = Buf("RK")
        DST = sb("DST", [128, 2], I32); bDST = Buf("DST")
        TOK = sb("TOK", [128, 4], F32); bTOK = Buf("TOK")
        bXS = Buf("XSdram")
        MEMX = X2; bMEMX = bX2
        MEMT = sb("MEMT", [128, 8, MEM], BF16); bMEMT = Buf("MEMT")

        CVB = MEMT[:].rearrange("p k m -> p (k m)")
        bCV = [Buf("CV0"), Buf("CV1")]
        cv_i = [0]
        PTR = ps("PTR", [128, 8, 128], BF16); bPTR = Buf("PTR")
        PA = ps("PA", [128, 512], F32); bPA = Buf("PA")
        PB = ps("PB", [128, 512], F32); bPB = Buf("PB")
        PS_ = [ps("PS%d" % i, [128, 512], F32) for i in range(2)]; bPS = [Buf("PS%d" % i) for i in range(2)]
        PO = [ps("PO%d" % i, [128, 512], F32) for i in range(2)]; bPO = [Buf("PO%d" % i) for i in range(2)]
        PM = ps("PM", [128, 512], F32); bPM = Buf("PM")

        op = sc.op

        def ld(dst, src, buf):
            op("sp", lambda e: e.dma_start(out=dst, in_=src), writes=[buf], dma=buf)

        ld(GK[:], gk_d, bGK); ld(SM[:], sm_d, bSM); ld(GFF[:], gff_d, bGFF)
        ld(CB[:], cb_d, bCB); ld(CF[:], cf_d, bCF); ld(AM[:], am_d, bAM); ld(POSI[:], pos_d, bPOSI)
        op("dve", lambda e: e.tensor_copy(out=POSF[:], in_=POSI[:]), reads=[bPOSI], writes=[bPOSF])
        ld(SM2[:], sm2_d, bSM2)
        op("dve", lambda e: e.tensor_tensor(out=STRI[:], in0=CB[:, 128:256], in1=CB[:, 0:128], op=ALU.subtract),
           reads=[bCB], writes=[bSTRI])
        op("dve", lambda e: e.tensor_copy(out=BASE[:], in_=SM2[:, 0:16]), reads=[bSM2], writes=[bBASE])

        stg_i = [0]

        def load_cast(dst, src, parts, cols, scale=None):
            i = stg_i[0] % 2
            stg_i[0] += 1
            op("sp", lambda e: e.dma_start(out=STG[i][0:parts, 0:cols], in_=src), writes=[bSTG[i]], dma=bSTG[i])
            if scale is None:
                op("dve", lambda e: e.tensor_copy(out=dst, in_=STG[i][0:parts, 0:cols]), reads=[bSTG[i]], writes=[bWIN])
            else:
                op("dve", lambda e: e.tensor_scalar(out=dst, in0=STG[i][0:parts, 0:cols], scalar1=scale, scalar2=None,
                                                    op0=ALU.mult), reads=[bSTG[i], bGK], writes=[bWIN])

        for kc in range(8):
            for (c0, cn) in ((0, 1024), (1024, 1024), (2048, 1032)):
                load_cast(WIN[:, kc, c0:c0 + cn], w_in_d[kc * 128:(kc + 1) * 128, c0:c0 + cn], 128, cn, GK[:, kc:kc + 1])
        for h in range(4):
            load_cast(WOA[:, h, :], w_out_d[h * 128:(h + 1) * 128, :], 128, D)
            load_cast(WOM[:, h, :], w_out_d[512 + h * 128:512 + (h + 1) * 128, :], 128, D)
            load_cast(WQM[:, h, :], wqm_d[h], 128, 128)
            load_cast(WKM[:, h, :], wkm_d[h], 128, 128)
        for h in range(2):
            load_cast(WOX[:, h, :], wox_d[h * 128:(h + 1) * 128, :], 128, D)
        for kc in range(8):
            load_cast(WQX[:, kc, :], wqx_d[kc * 128:(kc + 1) * 128, :], 128, 256, GK[:, 8 + kc:9 + kc])
            load_cast(WKV[:, kc, :], wkv_d[kc * 128:(kc + 1) * 128, :], 128, 512, GK[:, 16 + kc:17 + kc])
            ld(WR32[:, kc, :], wr_d[kc * 128:(kc + 1) * 128, :], bWR32)
        for b in (bWOA, bWOM, bWQX, bWKV, bWOX, bWQM, bWKM):
            b.w = bWIN.w
        op("dve", lambda e: e.tensor_copy(out=WRH[:], in_=WR32[:]), reads=[bWR32], writes=[bWRH])
        op("dve", lambda e: e.tensor_tensor(out=WRT[:], in0=WR32[:], in1=WRH[:], op=ALU.subtract),
           reads=[bWR32, bWRH], writes=[bWRT])
        op("dve", lambda e: e.tensor_copy(out=WRL[:], in_=WRT[:]), reads=[bWRT], writes=[bWRL])
        op("pool", lambda e: e.memset(VAR[:], 1.0), writes=bVAR)
        op("pool", lambda e: e.memset(VAM[:], 1.0), writes=[bVAM])
        op("pool", lambda e: e.memset(VXA[:], 1.0), writes=[bVXA])
        op("pool", lambda e: e.memset(MUB[:], 0.0), writes=[bMUB])
        op("pool", lambda e: e.memset(QT[:], 0.0), writes=[bQT])
        op("pool", lambda e: e.memset(CFS[:], 0.0), writes=[bCFS] + bCFSh)
        op("pool", lambda e: e.memset(CBS[:], 0.0), writes=[bCBS])

        def rms_stats(src, bsrc, col):
            op("act", lambda e: e.activation(out=XN[:], in_=src, func=AF.Square, accum_out=ST[:, col:col + 1]),
               reads=[bsrc], writes=[bXN, bST])
            op("dve", lambda e: e.tensor_scalar(out=ST[:, col:col + 1], in0=ST[:, col:col + 1], scalar1=1.0 / D,
                                                scalar2=EPS, op0=ALU.mult, op1=ALU.add), reads=[bST], writes=[bST])
            op("act", lambda e: e.activation(out=ST[:, col:col + 1], in_=ST[:, col:col + 1], func=AF.Ln), reads=[bST], writes=[bST])
            op("act", lambda e: e.activation(out=ST[:, col:col + 1], in_=ST[:, col:col + 1], func=AF.Exp, scale=-0.5),
               reads=[bST], writes=[bST])

        def transpose8(src, bsrc, dst, bdst, n=8, eng="dve"):
            for kc in range(n):
                op("pe", lambda e, kc=kc: e.transpose(out=PTR[:, kc, :], in_=src[:, kc * 128:(kc + 1) * 128],
                                                      identity=ident), reads=[bsrc, bCB], writes=[bPTR])
            if eng == "dve":
                op("dve", lambda e: e.tensor_copy(out=dst, in_=PTR[:, 0:n, :]), reads=[bPTR], writes=[bdst])
            else:
                op("act", lambda e: e.activation(out=dst, in_=PTR[:, 0:n, :], func=AF.Copy), reads=[bPTR], writes=[bdst])

        for mt in range(2):
            ld(MEMX[:], mem_d[mt * 128:(mt + 1) * 128, :], bMEMX)
            rms_stats(MEMX[:], bMEMX, 0)
            op("act", lambda e: e.activation(out=XN[:], in_=MEMX[:], func=AF.Copy, scale=ST[:, 0:1]),
               reads=[bMEMX, bST], writes=[bXN])
            transpose8(XN, bXN, MEMT[:, :, mt * 128:(mt + 1) * 128], bMEMT)
        for h in range(4):
            for kc in range(8):
                op("pe", lambda e, h=h, kc=kc: e.matmul(out=PA[0:64, 0:MEM], lhsT=WKV[:, kc, h * 64:(h + 1) * 64],
                                                        rhs=MEMT[:, kc, :], start=(kc == 0), stop=(kc == 7)),
                   reads=[bWKV, bMEMT], writes=[bPA])
            op("dve", lambda e, h=h: e.tensor_copy(out=KXT[:, h, :], in_=PA[0:64, 0:MEM]), reads=[bPA], writes=[bKXT])
        for mt in range(2):
            for kc in range(8):
                op("pe", lambda e, mt=mt, kc=kc: e.matmul(out=PA[:, 0:256], lhsT=MEMT[:, kc, mt * 128:(mt + 1) * 128],
                                                          rhs=WKV[:, kc, 256:512], start=(kc == 0), stop=(kc == 7)),
                   reads=[bWKV, bMEMT], writes=[bPA])
            op("dve", lambda e, mt=mt: e.tensor_copy(out=VXA[:, mt, :, 0:64],
                                                     in_=PA[:, 0:256].rearrange("p (h d) -> p h d", h=4)),
               reads=[bPA], writes=[bVXA])

        ld(XT[0][:], x_d[0:128, :], bXT[0])
        def do_chunk(c):
            xs = c % 2
            if c + 1 < NCH:
                ld(XT[(c + 1) % 3][:], x_d[(c + 1) * 128:(c + 2) * 128, :], bXT[(c + 1) % 3])
            X = XT[c % 3]; bX = bXT[c % 3]
            slot = c % NJ
            rms_stats(X[:], bX, 0)
            op("act", lambda e: e.activation(out=XN[:], in_=X[:], func=AF.Copy, scale=ST[:, 0:1]),
               reads=[bX, bST], writes=[bXN])
            transpose8(XN, bXN, HT[:], bHT)
            op("dve", lambda e, c=c: e.tensor_scalar(out=ROT[:, 1, :], in0=invE, scalar1=POSF[:, c:c + 1], scalar2=None,
                                                     op0=ALU.mult), reads=[bCF, bPOSF], writes=[bROT])
            op("dve", lambda e: e.tensor_scalar_add(out=ROT[:, 2, :], in0=ROT[:, 1, :], scalar1=0.5 * math.pi),
               reads=[bROT], writes=[bROT])
            op("dve", lambda e: e.tensor_scalar(out=ROTI[:], in0=ROT[:, 1:3, :], scalar1=1.0 / (2 * math.pi), scalar2=None,
                                                op0=ALU.mult), reads=[bROT], writes=[bROTI])
            op("dve", lambda e: e.tensor_copy(out=ROT[:, 3:5, :], in_=ROTI[:]), reads=[bROTI], writes=[bROT])
            op("dve", lambda e: e.scalar_tensor_tensor(out=ROT[:, 1:3, :], in0=ROT[:, 3:5, :], scalar=-2 * math.pi,
                                                       in1=ROT[:, 1:3, :], op0=ALU.mult, op1=ALU.add), reads=[bROT], writes=[bROT])
            op("dve", lambda e: e.tensor_single_scalar(out=ROT[:, 3:5, :], in_=ROT[:, 1:3, :], scalar=math.pi, op=ALU.is_gt),
               reads=[bROT], writes=[bROT])
            op("dve", lambda e: e.scalar_tensor_tensor(out=ROT[:, 1:3, :], in0=ROT[:, 3:5, :], scalar=-2 * math.pi,
                                                       in1=ROT[:, 1:3, :], op0=ALU.mult, op1=ALU.add), reads=[bROT], writes=[bROT])
            op("act", lambda e: e.activation(out=TRG[:], in_=ROT[:, 1:3, :], func=AF.Sin), reads=[bROT], writes=[bTRG])
            sinv = TRG[:, 0, :].rearrange("p (h i) -> p h i", h=8)
            cosv = TRG[:, 1, :].rearrange("p (h i) -> p h i", h=8)

            def proj_tok(pt, bpt, c0, n):
                for kc in range(8):
                    op("pe", lambda e, kc=kc: e.matmul(out=pt[:, 0:n], lhsT=HT[:, kc, :], rhs=WIN[:, kc, c0:c0 + n],
                                                       start=(kc == 0), stop=(kc == 7)), reads=[bHT, bWIN], writes=[bpt])

            def rope(pt, bpt, dst, bdst):
                pv = pt[:, :].rearrange("p (h d) -> p h d", h=8)
                dv = dst[:, :].rearrange("p (h d) -> p h d", h=8)
                r = [ROT[:, 3 + i, :].rearrange("p (h i) -> p h i", h=8) for i in range(3)]
                t1 = pv[:, :, 0:8]; t2 = pv[:, :, 8:16]
                op("dve", lambda e: e.tensor_tensor(out=r[0], in0=t1, in1=cosv, op=ALU.mult), reads=[bpt, bTRG], writes=[bROT])
                op("dve", lambda e: e.tensor_tensor(out=r[1], in0=t2, in1=sinv, op=ALU.mult), reads=[bpt, bTRG], writes=[bROT])
                op("dve", lambda e: e.tensor_tensor(out=dv[:, :, 0:8], in0=r[0], in1=r[1], op=ALU.subtract),
                   reads=[bROT], writes=[bdst])
                op("dve", lambda e: e.tensor_tensor(out=r[0], in0=t2, in1=cosv, op=ALU.mult), reads=[bpt, bTRG], writes=[bROT])
                op("dve", lambda e: e.tensor_tensor(out=r[1], in0=t1, in1=sinv, op=ALU.mult), reads=[bpt, bTRG], writes=[bROT])
                op("dve", lambda e: e.tensor_tensor(out=dv[:, :, 8:16], in0=r[0], in1=r[1], op=ALU.add),
                   reads=[bROT], writes=[bdst])
                op("act", lambda e: e.activation(out=dv[:, :, 16:64], in_=pv[:, :, 16:64], func=AF.Copy),
                   reads=[bpt], writes=[bdst])

            proj_tok(PA, bPA, 0, 512)
            rope(PA, bPA, QR, bQR)
            proj_tok(PB, bPB, 512, 512)
            rope(PB, bPB, KR, bKR)
            for kc in range(4):
                op("pe", lambda e, kc=kc: e.transpose(out=PTR[:, kc, :], in_=QR[:, kc * 128:(kc + 1) * 128], identity=ident),
                   reads=[bQR, bCB], writes=[bPTR])
            op("dve", lambda e: e.tensor_copy(out=QT[0:64, :, 0, :], in_=PTR[0:64, 0:4, :]), reads=[bPTR], writes=[bQT])
            op("act", lambda e: e.activation(out=QT[64:128, :, 1, :], in_=PTR[64:128, 0:4, :], func=AF.Copy), reads=[bPTR], writes=[bQT])
            transpose8(KR, bKR, KTR[:, :, slot * 128:(slot + 1) * 128], bKTR[slot], n=4, eng="act")
            proj_tok(PA, bPA, 1024, 512)
            op("act", lambda e: e.activation(out=VAR[:, slot, :, 0:64], in_=PA[:, :].rearrange("p (h d) -> p h d", h=8),
                                             func=AF.Copy), reads=[bPA], writes=[bVAR[slot]])
            sc.defer = True
            nsub = 64 // NCH
            for (srcv, dstv) in ((w1v, wb1_d), (w3v, wb3_d), (w2v, wb2_d)):
                for sub in range(nsub):
                    l0 = (c * nsub + sub) * 128
                    ci = cv_i[0] % 2
                    cv_i[0] += 1
                    op("sp", lambda e, ci=ci, l0=l0, srcv=srcv: e.dma_start(out=STG[ci][:, 0:1024], in_=srcv[l0:l0 + 128, :]),
                       writes=[bSTG[ci]], dma=bSTG[ci])
                    op("pool", lambda e, ci=ci: e.tensor_copy(out=CVB[:, ci * 1024:(ci + 1) * 1024], in_=STG[ci][:, 0:1024]),
                       reads=[bSTG[ci]], writes=[bCV[ci], bMEMT])
                    op("sp", lambda e, ci=ci, l0=l0, dstv=dstv: e.dma_start(out=dstv[l0:l0 + 128, :],
                                                                          in_=CVB[:, ci * 1024:(ci + 1) * 1024]),
                       reads=[bCV[ci]], dma=bCV[ci])
            proj_tok(PB, bPB, 3072, 8)
            for h in range(4):
                for kc in range(8):
                    op("pe", lambda e, h=h, kc=kc: e.matmul(out=PM[:, h * 128:(h + 1) * 128],
                                                            lhsT=WIN[:, kc, 1536 + h * 128:1536 + (h + 1) * 128],
                                                            rhs=HT[:, kc, :], start=(kc == 0), stop=(kc == 7)),
                       reads=[bHT, bWIN], writes=[bPM])
            op("dve", lambda e: e.tensor_copy(out=MUB[:, :, 3:131], in_=PM[:, :].rearrange("p (h t) -> p h t", h=4)),
               reads=[bPM], writes=[bMUB])
            for h in range(4):
                op("dve", lambda e, h=h: e.tensor_scalar(out=CPRE[:, h, :], in0=MUB[:, h, 0:128],
                                                         scalar1=SM[:, 8 + h * 4:9 + h * 4], scalar2=SM[:, 24 + h:25 + h],
                                                         op0=ALU.mult, op1=ALU.add), reads=[bMUB, bSM], writes=[bCPh[h]])
            for j in range(1, 4):
                for h in range(4):
                    op("dve", lambda e, h=h, j=j: e.scalar_tensor_tensor(
                        out=CPRE[:, h, :], in0=MUB[:, h, j:j + 128], scalar=SM[:, 8 + h * 4 + j:9 + h * 4 + j],
                        in1=CPRE[:, h, :], op0=ALU.mult, op1=ALU.add), reads=[bMUB, bSM, bCPh[h]], writes=[bCPh[h]])
            proj_tok(PA, bPA, 2048, 512)
            op("dve", lambda e: e.tensor_copy(out=VAM[:, :, 0:128], in_=PA[:, :].rearrange("p (h d) -> p h d", h=4)),
               reads=[bPA], writes=[bVAM])
            op("dve", lambda e: e.tensor_tensor(out=GT[:, 0:8], in0=PB[:, 0:8], in1=SM[:, 0:8], op=ALU.add),
               reads=[bPB, bSM], writes=[bGT])
            op("act", lambda e: e.activation(out=GT[:, 8:12], in_=GT[:, 4:8], func=AF.Exp, scale=-1.0),
               reads=[bGT], writes=[bGT])
            op("act", lambda e: e.activation(out=GT[:, 12:16], in_=GT[:, 8:12], func=AF.Ln, bias=1.0),
               reads=[bGT], writes=[bGT])
            op("pe", lambda e: e.matmul(out=PB[:, 16:20], lhsT=tri_f, rhs=GT[:, 12:16], start=True, stop=True),
               reads=[bGT, bCF], writes=[bPB])
            op("pe", lambda e: e.matmul(out=PB[:, 32:36], lhsT=ones_f, rhs=GT[:, 12:16], start=True, stop=True),
               reads=[bGT, bCF], writes=[bPB])
            op("dve", lambda e: e.tensor_copy(out=GT[:, 16:20], in_=PB[:, 16:20]), reads=[bPB], writes=[bGT])
            op("dve", lambda e: e.tensor_copy(out=GT[:, 20:24], in_=PB[:, 32:36]), reads=[bPB], writes=[bGT])
            op("dve", lambda e: e.tensor_tensor(out=GT[:, 40:44], in0=GT[:, 0:4], in1=GT[:, 16:20], op=ALU.add),
               reads=[bGT], writes=[bGT])
            op("dve", lambda e: e.tensor_tensor(out=GT[:, 44:48], in0=GT[:, 40:44], in1=GT[:, 20:24], op=ALU.subtract),
               reads=[bGT], writes=[bGT])
            op("act", lambda e: e.activation(out=GT[:, 24:32], in_=GT[:, 40:48], func=AF.Exp), reads=[bGT], writes=[bGT])
            op("act", lambda e: e.activation(out=GT[:, 32:36], in_=GT[:, 16:20], func=AF.Exp, scale=-1.0),
               reads=[bGT], writes=[bGT])
            op("act", lambda e: e.activation(out=GT[:, 36:40], in_=GT[:, 20:24], func=AF.Exp, scale=-1.0),
               reads=[bGT], writes=[bGT])
            op("dve", lambda e: e.tensor_scalar(out=GT[:, 48:52], in0=GT[:, 28:32], scalar1=128.0 ** -0.5, scalar2=None,
                                                op0=ALU.mult), reads=[bGT], writes=[bGT])
            op("act", lambda e: e.activation(out=CT[:], in_=CPRE[:], func=AF.Silu), reads=bCPh, writes=[bCT])
            op("pool", lambda e: e.tensor_copy(out=MUB[:, :, 0:3], in_=MUB[:, :, 128:131]), reads=[bMUB], writes=[bMUB])
            for h in range(4):
                op("pe", lambda e, h=h: e.matmul(out=PM[:, h * 128:(h + 1) * 128], lhsT=WQM[:, h, :], rhs=CT[:, h, :],
                                                 start=True, stop=True), reads=[bWQM, bCT], writes=[bPM])
            for h in range(4):
                op("pe", lambda e, h=h: e.matmul(out=PA[:, h * 128:(h + 1) * 128], lhsT=WKM[:, h, :], rhs=CT[:, h, :],
                                                 start=True, stop=True), reads=[bWKM, bCT], writes=[bPA])
            for h in range(4):
                op("pe", lambda e, h=h: e.matmul(out=PB[:, h * 128:(h + 1) * 128], lhsT=CT[:, h, :], rhs=WKM[:, h, :],
                                                 start=True, stop=True), reads=[bWKM, bCT], writes=[bPB])
            op("dve", lambda e: e.tensor_copy(out=MQT[:].rearrange("p h t -> p (h t)"), in_=PM[:, :]),
               reads=[bPM], writes=[bMQT])
            op("dve", lambda e: e.tensor_scalar(out=MKT[:].rearrange("p h t -> p (h t)"), in0=PA[:, :], scalar1=128.0 ** -0.5,
                                                scalar2=None, op0=ALU.mult), reads=[bPA], writes=[bMKT])
            for h in range(4):
                op("dve", lambda e, h=h: e.tensor_scalar(out=KG[:, h, :], in0=PB[:, h * 128:(h + 1) * 128],
                                                         scalar1=GT[:, 48 + h:49 + h], scalar2=None, op0=ALU.mult),
                   reads=[bPB, bGT], writes=[bKG])
            for h in range(4):
                op("pe", lambda e, h=h: e.matmul(out=PM[:, h * 128:(h + 1) * 128], lhsT=MKT[:, h, :], rhs=MQT[:, h, :],
                                                 start=True, stop=True), reads=[bMKT, bMQT], writes=[bPM])
            for h in range(4):
                op("dve", lambda e, h=h: e.scalar_tensor_tensor(out=STL4[:, h, :], in0=PM[:, h * 128:(h + 1) * 128],
                                                                scalar=GT[:, 24 + h:25 + h], in1=tri_b, op0=ALU.mult, op1=ALU.mult),
                   reads=[bPM, bGT, bCB], writes=[bSTLh[h]])
            for h in range(4):
                op("pe", lambda e, h=h: e.matmul(out=PA[:, h * 128:(h + 1) * 128], lhsT=STL4[:, h, :], rhs=VAM[:, h, 0:128],
                                                 start=True, stop=False), reads=[bSTLh[h], bVAM], writes=[bPA])
                op("pe", lambda e, h=h: e.matmul(out=PA[:, h * 128:(h + 1) * 128], lhsT=MQT[:, h, :], rhs=CBS[:, h, 0:128],
                                                 start=False, stop=True), reads=[bMQT, bCBS], writes=[bPA])
            for h in range(4):
                op("pe", lambda e, h=h: e.matmul(out=PB[:, h:h + 1], lhsT=STL4[:, h, :], rhs=VAM[:, h, 128:129],
                                                 start=True, stop=False), reads=[bSTLh[h], bVAM], writes=[bPB])
                op("pe", lambda e, h=h: e.matmul(out=PB[:, h:h + 1], lhsT=MQT[:, h, :], rhs=CBS[:, h, 128:129],
                                                 start=False, stop=True), reads=[bMQT, bCBS], writes=[bPB])
            for h in range(4):
                op("pe", lambda e, h=h: e.matmul(out=PM[:, h * 128:(h + 1) * 128], lhsT=KG[:, h, :], rhs=VAM[:, h, 0:128],
                                                 start=True, stop=True), reads=[bKG, bVAM], writes=[bPM])
            for h in range(4):
                op("pe", lambda e, h=h: e.matmul(out=PB[:, 8 + h:9 + h], lhsT=KG[:, h, :], rhs=VAM[:, h, 128:129],
                                                 start=True, stop=True), reads=[bKG, bVAM], writes=[bPB])
            for h in range(4):
                op("dve", lambda e, h=h: e.scalar_tensor_tensor(out=CFS[:, h, 0:128], in0=CFS[:, h, 0:128],
                                                                scalar=GT[:, 36 + h:37 + h], in1=PM[:, h * 128:(h + 1) * 128],
                                                                op0=ALU.mult, op1=ALU.add),
                   reads=[bCFSh[h], bGT, bPM], writes=[bCFSh[h]])
            for h in range(4):
                op("dve", lambda e, h=h: e.scalar_tensor_tensor(out=CFS[:, h, 128:129], in0=CFS[:, h, 128:129],
                                                                scalar=GT[:, 36 + h:37 + h], in1=PB[:, 8 + h:9 + h],
                                                                op0=ALU.mult, op1=ALU.add),
                   reads=[bCFSh[h], bGT, bPB], writes=[bCFSh[h]])
            op("dve", lambda e: e.tensor_copy(out=CBS[:], in_=CFS[:]), reads=bCFSh, writes=[bCBS])
            op("dve", lambda e: e.tensor_tensor(out=ST[:, 8:12], in0=PB[:, 0:4], in1=GT[:, 32:36], op=ALU.mult),
               reads=[bPB, bGT], writes=[bEP])
            op("dve", lambda e: e.tensor_scalar(out=ST[:, 12:16], in0=ST[:, 8:12], scalar1=-1.0, scalar2=None, op0=ALU.mult),
               reads=[bEP], writes=[bEP])
            op("dve", lambda e: e.tensor_tensor(out=ST[:, 8:12], in0=ST[:, 8:12], in1=ST[:, 12:16], op=ALU.max),
               reads=[bEP], writes=[bEP])
            op("dve", lambda e: e.tensor_scalar_max(out=ST[:, 8:12], in0=ST[:, 8:12], scalar1=1.0), reads=[bEP], writes=[bEP])
            op("dve", lambda e: e.reciprocal(out=ST[:, 16:20], in_=ST[:, 8:12]), reads=[bEP], writes=[bEP])
            op("dve", lambda e: e.tensor_tensor(out=ST[:, 16:20], in0=ST[:, 16:20], in1=GT[:, 32:36], op=ALU.mult),
               reads=[bEP, bGT], writes=[bEP])
            for h in range(4):
                op("act", lambda e, h=h: e.activation(out=SCR[:, 0:128], in_=PA[:, h * 128:(h + 1) * 128], func=AF.Square,
                                                      accum_out=ST[:, 20 + h:21 + h]), reads=[bPA], writes=[bSCR, bSSh[h]])
            op("dve", lambda e: e.tensor_tensor(out=ST[:, 24:28], in0=ST[:, 16:20], in1=ST[:, 16:20], op=ALU.mult),
               reads=[bEP], writes=[bEP])
            op("dve", lambda e: e.tensor_tensor(out=ST[:, 24:28], in0=ST[:, 24:28], in1=ST[:, 20:24], op=ALU.mult),
               reads=[bEP] + bSSh, writes=[bEP])
            op("dve", lambda e: e.tensor_scalar(out=ST[:, 24:28], in0=ST[:, 24:28], scalar1=1.0 / 128, scalar2=EPS,
                                                op0=ALU.mult, op1=ALU.add), reads=[bEP], writes=[bEP])
            op("act", lambda e: e.activation(out=ST[:, 24:28], in_=ST[:, 24:28], func=AF.Ln), reads=[bEP], writes=[bEP])
            op("act", lambda e: e.activation(out=ST[:, 24:28], in_=ST[:, 24:28], func=AF.Exp, scale=-0.5),
               reads=[bEP], writes=[bEP])
            op("dve", lambda e: e.tensor_tensor(out=ST[:, 24:28], in0=ST[:, 24:28], in1=ST[:, 16:20], op=ALU.mult),
               reads=[bEP], writes=[bEP])
            for h in range(4):
                op("dve", lambda e, h=h: e.tensor_scalar(out=HN[:, h, :], in0=PA[:, h * 128:(h + 1) * 128],
                                                         scalar1=ST[:, 24 + h:25 + h], scalar2=None, op0=ALU.mult),
                   reads=[bPA, bEP], writes=[bHN])
            for h in range(4):
                op("pe", lambda e, h=h: e.transpose(out=PTR[:, h, :], in_=HN[:, h, :], identity=ident),
                   reads=[bHN, bCB], writes=[bPTR])
            for h in range(4):
                for kc in range(8):
                    op("pe", lambda e, h=h, kc=kc: e.matmul(out=PM[:, h * 128:(h + 1) * 128],
                                                            lhsT=WIN[:, kc, 2560 + h * 128:2560 + (h + 1) * 128],
                                                            rhs=HT[:, kc, :], start=(kc == 0), stop=(kc == 7)),
                       reads=[bHT, bWIN], writes=[bPM])
            for h in range(4):
                op("dve", lambda e, h=h: e.tensor_scalar(out=YM[:, h, :], in0=CT[:, h, :], scalar1=SM[:, 32 + h:33 + h], scalar2=None,
                                                         op0=ALU.mult), reads=[bCT, bSM], writes=[bYMh[h]])
            for h in range(4):
                op("dve", lambda e, h=h: e.scalar_tensor_tensor(out=YM[:, h, :], in0=PTR[:, h, :], scalar=SM[:, 28 + h:29 + h],
                                                                in1=YM[:, h, :], op0=ALU.mult, op1=ALU.add),
                   reads=[bPTR, bSM, bYMh[h]], writes=[bYMh[h]])
            op("act", lambda e: e.activation(out=SGB[:], in_=PM[:, :], func=AF.Sigmoid), reads=[bPM], writes=[bSG])
            op("dve", lambda e: e.tensor_tensor(out=YM[:].rearrange("p h t -> p (h t)"), in0=YM[:].rearrange("p h t -> p (h t)"),
                                                in1=SGB[:], op=ALU.mult), reads=bYMh + [bSG], writes=[bYM] + bYMh)
            sc.defer = False
            nj = min(c, NJ - 1) + 1
            groups = []
            for h in range(8):
                js = list(range(nj))
                for g0 in range(0, nj, 4):
                    groups.append((h, js[g0:g0 + 4]))

            def rec_S(gi):
                h, jl = groups[gi]
                pb_ = gi % 2
                po = (h % 2) * 64
                n_ = len(jl) * 128
                j0_ = jl[0]
                op("pe", lambda e: e.matmul(out=PS_[pb_][:, 0:n_], lhsT=ident, rhs=AM[:, j0_ * 128:j0_ * 128 + n_],
                                            start=True, stop=False), reads=[bCB, bAM], writes=[bPS[pb_]])
                for jj, j in enumerate(jl):
                    ks = (c - j) % NJ
                    op("pe", lambda e, jj=jj, ks=ks: e.matmul(
                        out=PS_[pb_][:, jj * 128:(jj + 1) * 128],
                        lhsT=KTR[:, h // 2, ks * 128:(ks + 1) * 128],
                        rhs=QT[:, h // 2, h % 2, :], start=False, stop=(jj == len(jl) - 1)),
                       reads=[bKTR[ks], bQT], writes=[bPS[pb_]])

            def rec_EM(gi):
                h, jl = groups[gi]
                pb_ = gi % 2
                n = len(jl) * 128
                j0 = jl[0]
                op("act", lambda e: e.activation(out=PT[pb_][:, 0:n], in_=PS_[pb_][:, 0:n], func=AF.Exp, scale=0.125),
                   reads=[bPS[pb_]], writes=[bPT[pb_]])

            def rec_PV(gi):
                h, jl = groups[gi]
                pb_ = gi % 2
                for jj, j in enumerate(jl):
                    ks = (c - j) % NJ
                    op("pe", lambda e, jj=jj, ks=ks, j=j: e.matmul(
                        out=PO[h // 4][:, (h % 4) * 65:(h % 4 + 1) * 65],
                        lhsT=PT[pb_][:, jj * 128:(jj + 1) * 128], rhs=VAR[:, ks, h, :],
                        start=(j == 0), stop=(j == nj - 1)),
                       reads=[bVAR[ks], bPT[pb_]], writes=[bPO[h // 4]])

            per = -(-len(sc.deferq) // len(groups))
            rec_S(0)
            for gi in range(len(groups)):
                if gi + 1 < len(groups):
                    rec_S(gi + 1)
                rec_EM(gi)
                rec_PV(gi)
                sc.flush(per)
            sc.flush()
            for hh in range(2):
                op("dve", lambda e, hh=hh: e.reciprocal(
                    out=RDS[:, hh * 4:(hh + 1) * 4, :],
                    in_=PO[hh][:, 0:260].rearrange("p (h d) -> p h d", h=4)[:, :, 64:65]), reads=[bPO[hh]], writes=[bRD])
            for h in range(8):
                op("act", lambda e, h=h: e.activation(out=YAT[:, h * 64:(h + 1) * 64],
                                                      in_=PO[h // 4][:, (h % 4) * 65:(h % 4) * 65 + 64], func=AF.Copy,
                                                      scale=RDS[:, h, :]), reads=[bPO[h // 4], bRD], writes=[bYAT])
            transpose8(YAT, bYAT, YA[:], bYA, n=4)

            sc.defer = True
            for half, (pt, bpt) in enumerate(((PA, bPA), (PB, bPB))):
                cs = slice(half * 512, (half + 1) * 512)
                for h in range(4):
                    op("pe", lambda e, h=h, pt=pt, cs=cs: e.matmul(out=pt[:, :], lhsT=YA[:, h, :], rhs=WOA[:, h, cs],
                                                                  start=(h == 0), stop=False), reads=[bYA, bWOA], writes=[bpt])
                for h in range(4):
                    op("pe", lambda e, h=h, pt=pt, cs=cs: e.matmul(out=pt[:, :], lhsT=YM[:, h, :], rhs=WOM[:, h, cs],
                                                                  start=False, stop=(h == 3)), reads=[bYM, bWOM], writes=[bpt])
                op("dve", lambda e, pt=pt, cs=cs: e.tensor_tensor(out=X2[:, cs], in0=pt[:, :], in1=X[:, cs], op=ALU.add),
                   reads=[bpt, bX], writes=[bX2])
            if debug:
                op("sp", lambda e, c=c: e.dma_start(out=dbg2_d[c * 128:(c + 1) * 128, :], in_=X2[:]), reads=[bX2], dma=bX2)
            rms_stats(X2[:], bX2, 1)
            op("dve", lambda e: e.tensor_scalar(out=XN[:], in0=X2[:], scalar1=ST[:, 1:2], scalar2=None, op0=ALU.mult),
               reads=[bX2, bST], writes=[bXN])
            transpose8(XN, bXN, H2T[:], bH2T)
            for h in range(4):
                for kc in range(8):
                    op("pe", lambda e, h=h, kc=kc: e.matmul(out=PM[0:64, h * 128:(h + 1) * 128],
                                                            lhsT=WQX[:, kc, h * 64:(h + 1) * 64], rhs=H2T[:, kc, :],
                                                            start=(kc == 0), stop=(kc == 7)), reads=[bWQX, bH2T], writes=[bPM])
            op("dve", lambda e: e.tensor_copy(out=QXT[:].rearrange("p h t -> p (h t)"), in_=PM[0:64, :]),
               reads=[bPM], writes=[bQXT])
            for h in range(4):
                for mt in range(2):
                    i = h * 2 + mt
                    op("pe", lambda e, h=h, mt=mt, i=i: e.matmul(out=(PA, PB)[i // 4][:, (i % 4) * 128:(i % 4 + 1) * 128],
                                                                 lhsT=KXT[:, h, mt * 128:(mt + 1) * 128], rhs=QXT[:, h, :],
                                                                 start=True, stop=True), reads=[bKXT, bQXT], writes=[(bPA, bPB)[i // 4]])
            for b2 in range(2):
                op("act", lambda e, b2=b2: e.activation(out=PXT[:, b2 * 4:(b2 + 1) * 4, :].rearrange("p a t -> p (a t)"),
                                                        in_=(PA, PB)[b2][:, :], func=AF.Exp, scale=0.125),
                   reads=[(bPA, bPB)[b2]], writes=[bPXT])
            for h in range(4):
                for mt in range(2):
                    op("pe", lambda e, h=h, mt=mt: e.matmul(out=PM[:, h * 65:(h + 1) * 65], lhsT=PXT[:, h * 2 + mt, :],
                                                            rhs=VXA[:, mt, h, :], start=(mt == 0), stop=(mt == 1)),
                       reads=[bVXA, bPXT], writes=[bPM])
            op("dve", lambda e: e.reciprocal(out=RDX[:, 0:4, :],
                                             in_=PM[:, 0:260].rearrange("p (h d) -> p h d", h=4)[:, :, 64:65]),
               reads=[bPM], writes=[bRDX])
            for h in range(4):
                op("dve", lambda e, h=h: e.tensor_scalar(out=OXK[:, h * 64:(h + 1) * 64], in0=PM[:, h * 65:h * 65 + 64],
                                                         scalar1=RDX[:, h, :], scalar2=None, op0=ALU.mult),
                   reads=[bPM, bRDX], writes=[bOXK])
            transpose8(OXK, bOXK, OXT[:], bOXT, n=2)
            X3c = X3[xs]; bX3c = bX3[xs]
            for half, (pt, bpt) in enumerate(((PA, bPA), (PB, bPB))):
                cs = slice(half * 512, (half + 1) * 512)
                for h in range(2):
                    op("pe", lambda e, h=h, pt=pt, cs=cs: e.matmul(out=pt[:, :], lhsT=OXT[:, h, :], rhs=WOX[:, h, cs],
                                                                  start=(h == 0), stop=(h == 1)), reads=[bOXT, bWOX], writes=[bpt])
                op("dve", lambda e, pt=pt, cs=cs: e.tensor_tensor(out=X3c[:, cs], in0=pt[:, :], in1=X2[:, cs], op=ALU.add),
                   reads=[bpt, bX2], writes=[bX3c])
            op("sp", lambda e, c=c: e.dma_start(out=out_d[c * 128:(c + 1) * 128, :], in_=X3c[:]), reads=[bX3c], dma=bX3c)
            if debug:
                op("sp", lambda e, c=c: e.dma_start(out=dbg3_d[c * 128:(c + 1) * 128, :], in_=X3c[:]), reads=[bX3c], dma=bX3c)
            rms_stats(X3c[:], bX3c, 2)
            op("dve", lambda e: e.scalar_tensor_tensor(out=XN3[:], in0=X3c[:], scalar=ST[:, 2:3], in1=GFF[:], op0=ALU.mult,
                                                       op1=ALU.mult), reads=[bX3c, bST, bGFF], writes=[bXN3])
            op("dve", lambda e: e.tensor_copy(out=HI[:], in_=XN3[:]), reads=[bXN3], writes=[bHI])
            op("dve", lambda e: e.tensor_tensor(out=LO[:], in0=XN3[:], in1=HI[:], op=ALU.subtract),
               reads=[bXN3, bHI], writes=[bLO])
            transpose8(HI, bHI, H3T[xs][:], bH3T[xs])
            transpose8(LO, bLO, L3T[:], bL3T)
            op("sp", lambda e, c=c: e.dma_start(out=h3T_d[:, :, c * 128:(c + 1) * 128].rearrange("k p t -> p k t"),
                                                in_=H3T[xs][:]), reads=[bH3T[xs]], dma=bH3T[xs])
            n_mm = 0
            for (lt, blt, wt, bwt) in ((H3T[xs], bH3T[xs], WRH, bWRH), (L3T, bL3T, WRH, bWRH), (H3T[xs], bH3T[xs], WRL, bWRL)):
                for kc in range(8):
                    op("pe", lambda e, lt=lt, wt=wt, kc=kc, n_mm=n_mm: e.matmul(out=PM[:, 0:20], lhsT=lt[:, kc, :], rhs=wt[:, kc, :],
                                                                              start=(n_mm == 0), stop=(n_mm == 23)),
                       reads=[blt, bwt], writes=[bPM])
                    n_mm += 1
            op("dve", lambda e: e.tensor_tensor(out=RT[:, 0:20], in0=PM[:, 0:20], in1=SM[:, 36:56], op=ALU.add),
               reads=[bPM, bSM], writes=[bRT])
            op("sp", lambda e, c=c: e.dma_start(out=lg_d[c * 128:(c + 1) * 128, :], in_=RT[:, 0:20]), reads=[bRT], dma=bRT)
            op("sp", lambda e, c=c: e.dma_start(out=h3_d[c * 128:(c + 1) * 128, :], in_=HI[:]), reads=[bHI], dma=bHI)
            sc.defer = False

        for c in range(NCH):
            do_chunk(c)
        sc.flush()
        sc.finish("sp")
        sc.emit()

    with ExitStack() as st:
        sc = Sched(nc, semstack, "B")
        op = sc.op

        def sb(name, shape, dt):
            return st.enter_context(nc.sbuf_tensor(name, list(shape), dt))

        def ps(name, shape, dt):
            return st.enter_context(nc.psum_tensor(name, list(shape), dt))

        GFN = sb("bGFN", [128, D], F32); bGFN = Buf("bGFN", ro=True)
        SM2 = sb("bSM2", [128, 48], F32); bSM2 = Buf("bSM2", ro=True)
        IDN = sb("bIDN", [128, 128], BF16); bIDN = Buf("bIDN", ro=True)
        CNT = sb("bCNT", [128, NE], F32); bCNT = Buf("bCNT")
        NBf = sb("bNB", [128, NE], F32); bNB = Buf("bNB")
        NBi = sb("bNBi", [128, NE], I32); bNBi = Buf("bNBi")
        CUM = sb("bCUM", [128, NE], F32); bCUM = Buf("bCUM")
        MSK = sb("bMSK", [128, 2 * NE], F32); bMSK = Buf("bMSK")
        EJ = sb("bEJ", [128, NBLK], F32); bEJ = Buf("bEJ")
        IJ = sb("bIJ", [128, NBLK], F32); bIJ = Buf("bIJ")
        PST = sb("bPST", [128, NE], F32); bPST = Buf("bPST")
        DPF = sb("bDPF", [128, NCH, 2], F32); bDPF = Buf("bDPF")
        DPI = sb("bDPI", [128, NCH, 2], I32); bDPI = Buf("bDPI")
        WAB = sb("bWAB", [128, NCH, 2], F32); bWAB = Buf("bWAB")
        DEC = sb("bDEC", [128, 16], F32); bDEC = Buf("bDEC")
        DECI = sb("bDECI", [128, 2], I32); bDECI = Buf("bDECI")
        H3L = [sb("bH3L%d" % i, [128, D], BF16) for i in range(2)]; bH3L = [Buf("bH3L%d" % i) for i in range(2)]
        EW = sb("bEW", [128, 2, NBLK], F32); bEW = Buf("bEW")
        IXF = sb("bIXF", [128, NBLK, 16], F32); bIXF = Buf("bIXF")
        IXI = sb("bIXI", [128, NBLK, 16], I32); bIXI = Buf("bIXI", ro=True)
        W1 = [sb("bW1_%d" % i, [128, 8, FF], BF16) for i in range(2)]
        W3 = [sb("bW3_%d" % i, [128, 8, FF], BF16) for i in range(2)]
        W2 = [sb("bW2_%d" % i, [128, 4, D], BF16) for i in range(2)]
        bW = [Buf("bW%d" % i) for i in range(2)]
        XB = [sb("bXB%d" % i, [128, 4, D], BF16) for i in range(2)]; bXB = [Buf("bXB%d" % i) for i in range(2)]
        XBT = [sb("bXBT%d" % i, [128, 8, 512], BF16) for i in range(2)]; bXBT = [Buf("bXBT%d" % i) for i in range(2)]
        SA = [sb("bSA%d" % i, [128, 512], BF16) for i in range(2)]; bSA = [Buf("bSA%d" % i) for i in range(2)]
        HID = [sb("bHID%d" % i, [128, 4, 512], BF16) for i in range(2)]; bHID = [Buf("bHID%d" % i) for i in range(2)]
        YB = [sb("bYB%d" % i, [128, D], BF16) for i in range(3)]; bYB = [Buf("bYB%d" % i) for i in range(3)]
        X3L = [sb("bX3L%d" % i, [128, D], F32) for i in range(2)]; bX3L = [Buf("bX3L%d" % i) for i in range(2)]
        Y1 = [sb("bY1_%d" % i, [128, 2, D], BF16) for i in range(2)]; bY1 = [Buf("bY1_%d" % i) for i in range(2)]
        TK = [sb("bTK%d" % i, [128, 4], F32) for i in range(2)]; bTK = [Buf("bTK%d" % i) for i in range(2)]
        TKI = [sb("bTKI%d" % i, [128, 2], I32) for i in range(2)]; bTKI = [Buf("bTKI%d" % i) for i in range(2)]
        ST2 = sb("bST2", [128, 8], F32); bST2 = Buf("bST2")
        SC2 = sb("bSC2", [128, D], F32); bSC2 = Buf("bSC2")
        OUT = [sb("bOUT%d" % i, [128, D], F32) for i in range(2)]; bOUT = [Buf("bOUT%d" % i) for i in range(2)]
        QA = [ps("qA%d" % i, [128, 512], F32) for i in range(2)]; bQA = [Buf("qA%d" % i) for i in range(2)]
        QB = [ps("qB%d" % i, [128, 512], F32) for i in range(2)]; bQB = [Buf("qB%d" % i) for i in range(2)]
        QO = [ps("qO%d" % i, [128, 512], F32) for i in range(3)]; bQO = [Buf("qO%d" % i) for i in range(3)]
        QT_ = ps("qT", [128, 8, 128], BF16); bQT_ = Buf("qT")

        gate_ev = op("sp", lambda e: e.dma_start(out=GFN[:], in_=gfin_d), writes=[bGFN], dma=bGFN)
        for en in ("pe", "act", "dve", "pool"):
            sc._wait(en, gate_ev)
        op("sp", lambda e: e.dma_start(out=SM2[:], in_=sm2_d), writes=[bSM2], dma=bSM2)
        op("sp", lambda e: e.dma_start(out=IDN[:], in_=cb_d[:, 0:128]), writes=[bIDN], dma=bIDN)
        def T2(name, shape, dt=F32):
            return sb(name, shape, dt), Buf(name)
        LG, bLG = T2("rLG", [128, NCH, 20])
        GM, bGM = T2("rGM", [128, NCH])
        MG, bMG = T2("rMG", [128, 4, NCH])
        DG, bDG = T2("rDG", [128, 4, NCH])
        GS, bGS = T2("rGS", [128, NCH])
        EM, bEM = T2("rEM", [128, NCH, 16])
        EM2, bEM2 = T2("rEM2", [128, NCH, 16])
        MK1, bMK1 = T2("rMK1", [128, NCH, 16])
        MK2, bMK2 = T2("rMK2", [128, NCH, 16])
        M1, bM1 = T2("rM1", [128, NCH])
        M2, bM2 = T2("rM2", [128, NCH])
        PP, bPP = T2("rPP", [128, 2, NCH])
        ASGA, bASGA = T2("rASG", [128, NCH * 16], BF16)
        CSA, bCSA = T2("rCS", [128, NCH * 16])
        INA, bINA = T2("rINA", [128, NCH * 16])
        INB, bINB = T2("rINB", [128, NCH * 16])
        RKA, bRKA = T2("rRK", [128, NCH, 16])
        ONB, bONB = T2("rONB", [128, 128], BF16)
        STB, bSTB = T2("rSTB", [128, 128], BF16)
        op("sp", lambda e: e.dma_start(out=LG[:], in_=lg_d.rearrange("(n p) f -> p n f", p=128)), writes=[bLG], dma=bLG)
        op("sp", lambda e: e.dma_start(out=ONB[:], in_=cb_d[:, 256:384]), writes=[bONB], dma=bONB)
        op("sp", lambda e: e.dma_start(out=STB[:], in_=cb_d[:, 128:256]), writes=[bSTB], dma=bSTB)
        op("dve", lambda e: e.tensor_tensor(out=STB[:], in0=STB[:], in1=IDN[:], op=ALU.subtract), reads=[bSTB, bIDN], writes=[bSTB])
        op("dve", lambda e: e.reduce_max(out=GM[:], in_=LG[:, :, 0:4], axis=AX.X), reads=[bLG], writes=[bGM])
        for g in range(4):
            op("dve", lambda e, g=g: e.tensor_tensor(out=MG[:, g, :], in0=LG[:, :, g], in1=GM[:], op=ALU.is_ge),
               reads=[bLG, bGM], writes=[bMG])
            op("dve", lambda e, g=g: e.tensor_tensor(out=DG[:, g, :], in0=LG[:, :, g], in1=GM[:], op=ALU.subtract),
               reads=[bLG, bGM], writes=[bDG])
        op("act", lambda e: e.activation(out=DG[:], in_=DG[:], func=AF.Exp), reads=[bDG], writes=[bDG])
        op("dve", lambda e: e.tensor_tensor(out=GS[:], in0=DG[:, 0, :], in1=DG[:, 1, :], op=ALU.add), reads=[bDG], writes=[bGS])
        op("dve", lambda e: e.tensor_tensor(out=GS[:], in0=GS[:], in1=DG[:, 2, :], op=ALU.add), reads=[bDG, bGS], writes=[bGS])
        op("dve", lambda e: e.tensor_tensor(out=GS[:], in0=GS[:], in1=DG[:, 3, :], op=ALU.add), reads=[bDG, bGS], writes=[bGS])
        op("dve", lambda e: e.reciprocal(out=GS[:], in_=GS[:]), reads=[bGS], writes=[bGS])
        op("dve", lambda e: e.tensor_scalar(out=DG[:], in0=MG[:], scalar1=-1.0, scalar2=1e9, op0=ALU.add, op1=ALU.mult),
           reads=[bMG], writes=[bDG])
        for k in range(16):
            op("dve", lambda e, k=k: e.tensor_tensor(out=EM[:, :, k], in0=LG[:, :, 4 + k], in1=DG[:, k // 4, :], op=ALU.add),
               reads=[bLG, bDG], writes=[bEM])
        op("dve", lambda e: e.reduce_max(out=M1[:], in_=EM[:], axis=AX.X), reads=[bEM], writes=[bM1])
        for k in range(16):
            op("dve", lambda e, k=k: e.tensor_tensor(out=MK1[:, :, k], in0=EM[:, :, k], in1=M1[:], op=ALU.is_ge),
               reads=[bEM, bM1], writes=[bMK1])
        op("dve", lambda e: e.scalar_tensor_tensor(out=EM2[:].rearrange("p n k -> p (n k)"), in0=MK1[:].rearrange("p n k -> p (n k)"),
                                                   scalar=-1e9, in1=EM[:].rearrange("p n k -> p (n k)"), op0=ALU.mult, op1=ALU.add),
           reads=[bMK1, bEM], writes=[bEM2])
        op("dve", lambda e: e.reduce_max(out=M2[:], in_=EM2[:], axis=AX.X), reads=[bEM2], writes=[bM2])
        for k in range(16):
            op("dve", lambda e, k=k: e.tensor_tensor(out=MK2[:, :, k], in0=EM2[:, :, k], in1=M2[:], op=ALU.is_ge),
               reads=[bEM2, bM2], writes=[bMK2])
        op("dve", lambda e: e.tensor_tensor(out=PP[:, 0, :], in0=M2[:], in1=M1[:], op=ALU.subtract), reads=[bM1, bM2], writes=[bPP])
        op("act", lambda e: e.activation(out=PP[:, 0, :], in_=PP[:, 0, :], func=AF.Exp), reads=[bPP], writes=[bPP])
        op("dve", lambda e: e.tensor_scalar_add(out=PP[:, 0, :], in0=PP[:, 0, :], scalar1=1.0), reads=[bPP], writes=[bPP])
        op("dve", lambda e: e.reciprocal(out=PP[:, 0, :], in_=PP[:, 0, :]), reads=[bPP], writes=[bPP])
        op("dve", lambda e: e.tensor_scalar(out=PP[:, 1, :], in0=PP[:, 0, :], scalar1=-1.0, scalar2=1.0, op0=ALU.mult, op1=ALU.add),
           reads=[bPP], writes=[bPP])
        for k in range(2):
            op("dve", lambda e, k=k: e.tensor_tensor(out=WAB[:, :, k], in0=PP[:, k, :], in1=GS[:], op=ALU.mult),
               reads=[bPP, bGS], writes=[bWAB])
        op("dve", lambda e: e.tensor_tensor(out=ASGA[:], in0=MK1[:].rearrange("p n k -> p (n k)"),
                                            in1=MK2[:].rearrange("p n k -> p (n k)"), op=ALU.add), reads=[bMK1, bMK2], writes=[bASGA])
        NH = (NCH * 16 + 511) // 512
        for hf in range(NH):
            w = min(512, NCH * 16 - hf * 512)
            op("pe", lambda e, hf=hf, w=w: e.matmul(out=QA[hf % 2][:, 0:w], lhsT=ONB[:], rhs=ASGA[:, hf * 512:hf * 512 + w],
                                                    start=True, stop=True), reads=[bONB, bASGA], writes=[bQA[hf % 2]])
            op("dve", lambda e, hf=hf, w=w: e.tensor_copy(out=CSA[:, hf * 512:hf * 512 + w], in_=QA[hf % 2][:, 0:w]),
               reads=[bQA[hf % 2]], writes=[bCSA])
            op("pe", lambda e, hf=hf, w=w: e.matmul(out=QB[hf % 2][:, 0:w], lhsT=STB[:], rhs=ASGA[:, hf * 512:hf * 512 + w],
                                                    start=True, stop=True), reads=[bSTB, bASGA], writes=[bQB[hf % 2]])
            op("dve", lambda e, hf=hf, w=w: e.tensor_copy(out=RKA[:].rearrange("p n k -> p (n k)")[:, hf * 512:hf * 512 + w],
                                                          in_=QB[hf % 2][:, 0:w]), reads=[bQB[hf % 2]], writes=[bRKA])
        op("dve", lambda e: e.tensor_copy(out=INA[:], in_=CSA[:]), reads=[bCSA], writes=[bINA])
        cur, bcur, nxt, bnxt = INA, bINA, INB, bINB
        sft = 1
        while sft < NCH:
            o = sft * 16
            op("dve", lambda e, cur=cur, nxt=nxt, o=o: e.tensor_tensor(out=nxt[:, o:], in0=cur[:, o:], in1=cur[:, 0:NCH * 16 - o],
                                                                      op=ALU.add), reads=[bcur], writes=[bnxt])
            op("dve", lambda e, cur=cur, nxt=nxt, o=o: e.tensor_copy(out=nxt[:, 0:o], in_=cur[:, 0:o]), reads=[bcur], writes=[bnxt])
            cur, bcur, nxt, bnxt = nxt, bnxt, cur, bcur
            sft *= 2
        INC, bINC = cur, bcur
        op("dve", lambda e: e.tensor_tensor(out=CSA[:], in0=INC[:], in1=CSA[:], op=ALU.subtract), reads=[bINC, bCSA], writes=[bCSA])
        op("dve", lambda e: e.tensor_tensor(out=RKA[:].rearrange("p n k -> p (n k)"), in0=RKA[:].rearrange("p n k -> p (n k)"),
                                            in1=CSA[:], op=ALU.add), reads=[bRKA, bCSA], writes=[bRKA])
        op("dve", lambda e: e.tensor_copy(out=CNT[:], in_=INC[:, (NCH - 1) * 16:NCH * 16]), reads=[bINC], writes=[bCNT])
        op("dve", lambda e: e.tensor_scalar(out=CNT[:], in0=CNT[:], scalar1=float(BS - 1), scalar2=1.0 / BS,
                                            op0=ALU.add, op1=ALU.mult), reads=[bCNT], writes=[bCNT])
        op("dve", lambda e: e.tensor_copy(out=NBi[:], in_=CNT[:]), reads=[bCNT], writes=[bNBi])
        op("dve", lambda e: e.tensor_copy(out=NBf[:], in_=NBi[:]), reads=[bNBi], writes=[bNB])
        op("dve", lambda e: e.tensor_tensor(out=MSK[:, 0:16], in0=NBf[:], in1=CNT[:], op=ALU.is_gt),
           reads=[bNB, bCNT], writes=[bMSK])
        op("dve", lambda e: e.tensor_tensor(out=NBf[:], in0=NBf[:], in1=MSK[:, 0:16], op=ALU.subtract),
           reads=[bNB, bMSK], writes=[bNB])
        op("dve", lambda e: e.tensor_copy(out=CUM[:, 0:1], in_=NBf[:, 0:1]), reads=[bNB], writes=[bCUM])
        for e_ in range(1, NE):
            op("dve", lambda e, e_=e_: e.tensor_tensor(out=CUM[:, e_:e_ + 1], in0=CUM[:, e_ - 1:e_], in1=NBf[:, e_:e_ + 1],
                                                       op=ALU.add), reads=[bCUM, bNB], writes=[bCUM])
        for j in range(NBLK):
            op("dve", lambda e, j=j: e.tensor_single_scalar(out=MSK[:, 0:16], in_=CUM[:], scalar=float(j), op=ALU.is_le),
               reads=[bCUM], writes=[bMSK])
            op("dve", lambda e, j=j: e.reduce_sum(out=EJ[:, j:j + 1], in_=MSK[:, 0:16], axis=AX.X), reads=[bMSK], writes=[bEJ])
        op("dve", lambda e: e.tensor_scalar_min(out=EJ[:], in0=EJ[:], scalar1=float(NE - 1)), reads=[bEJ], writes=[bEJ])
        op("dve", lambda e: e.tensor_scalar(out=EW[:, 0, :], in0=EJ[:], scalar1=128.0, scalar2=SM2[:, 16:17],
                                            op0=ALU.mult, op1=ALU.add), reads=[bEJ, bSM2], writes=[bEW])
        op("dve", lambda e: e.tensor_scalar(out=EW[:, 1, :], in0=EJ[:], scalar1=float(FF), scalar2=SM2[:, 16:17],
                                            op0=ALU.mult, op1=ALU.add), reads=[bEJ, bSM2], writes=[bEW])
        for s_ in range(4):
            op("dve", lambda e, s_=s_: e.tensor_scalar_add(out=IXF[:, :, 12 + s_], in0=EW[:, 1, :], scalar1=float(s_ * 128)),
               reads=[bEW], writes=[bIXF])
        for kc in range(8):
            op("dve", lambda e, kc=kc: e.tensor_scalar_add(out=IXF[:, :, 4 + kc], in0=EW[:, 0, :], scalar1=0.0),
               reads=[bEW], writes=[bIXF])
        op("dve", lambda e: e.tensor_copy(out=IXI[:, :, 4:16], in_=IXF[:, :, 4:16]), reads=[bIXF], writes=[bIXI])
        op("dve", lambda e: e.tensor_tensor(out=PST[:], in0=CUM[:], in1=NBf[:], op=ALU.subtract), reads=[bCUM, bNB], writes=[bPST])
        op("dve", lambda e: e.tensor_scalar(out=PST[:], in0=PST[:], scalar1=float(BS), scalar2=None, op0=ALU.mult),
           reads=[bPST], writes=[bPST])
        for e_ in range(NE):
            op("dve", lambda e, e_=e_: e.tensor_scalar(out=RKA[:, :, e_], in0=RKA[:, :, e_], scalar1=PST[:, e_:e_ + 1], scalar2=None,
                                                       op0=ALU.add), reads=[bRKA, bPST], writes=[bRKA])
        op("dve", lambda e: e.tensor_tensor(out=EM[:].rearrange("p n k -> p (n k)"), in0=MK1[:].rearrange("p n k -> p (n k)"),
                                            in1=RKA[:].rearrange("p n k -> p (n k)"), op=ALU.mult), reads=[bMK1, bRKA], writes=[bEM])
        op("dve", lambda e: e.reduce_sum(out=M1[:], in_=EM[:], axis=AX.X), reads=[bEM], writes=[bM1])
        op("dve", lambda e: e.tensor_tensor(out=EM2[:].rearrange("p n k -> p (n k)"), in0=MK2[:].rearrange("p n k -> p (n k)"),
                                            in1=RKA[:].rearrange("p n k -> p (n k)"), op=ALU.mult), reads=[bMK2, bRKA], writes=[bEM2])
        op("dve", lambda e: e.reduce_sum(out=M2[:], in_=EM2[:], axis=AX.X), reads=[bEM2], writes=[bM2])
        op("dve", lambda e: e.tensor_copy(out=DPI[:, :, 0], in_=M1[:]), reads=[bM1], writes=[bDPI])
        op("dve", lambda e: e.tensor_copy(out=DPI[:, :, 1], in_=M2[:]), reads=[bM2], writes=[bDPI])
        for t in range(NCH):
            i = t % 2
            op("sp", lambda e, t=t, i=i: e.dma_start(out=H3L[i][:], in_=h3_d[t * 128:(t + 1) * 128, :]), writes=[bH3L[i]], dma=bH3L[i])
            for k in range(2):
                op("pool", lambda e, t=t, k=k, i=i: e.indirect_dma_start(
                    out=xs_d[:, :], out_offset=bass.IndirectOffsetOnAxis(ap=DPI[:, t, k:k + 1], axis=0),
                    in_=H3L[i][:], in_offset=None), reads=[bH3L[i], bDPI], dma=bH3L[i])
        for key, cnt_ in list(sc.dcnt.items()):
            sc._wait("sp", (key, 16 * cnt_))

        w1f = wb1_d.rearrange("(l a) c -> l (a c)", a=4)
        w3f = wb3_d.rearrange("(l a) c -> l (a c)", a=4)
        w2f = wb2_d.rearrange("(l a) c -> l (a c)", a=4)

        def gat(dst, src, j, col, buf):
            op("pool", lambda e: e.indirect_dma_start(out=dst, out_offset=None, in_=src,
                                                      in_offset=bass.IndirectOffsetOnAxis(ap=IXI[:, j, col:col + 1], axis=0)),
               reads=[bIXI], writes=[buf], dma=buf)

        def load_block(j):
            i = j % 2
            op("sp", lambda e: e.dma_start(out=XB[i][:], in_=xs_d[j * BS:(j + 1) * BS, :].rearrange("(s p) d -> p s d", p=128)),
               writes=[bXB[i]], dma=bXB[i])
            gat(W1[i][:].rearrange("p k f -> p (k f)"), w1f, j, 4, bW[i])
            gat(W3[i][:].rearrange("p k f -> p (k f)"), w3f, j, 4, bW[i])
            gat(W2[i][:].rearrange("p k f -> p (k f)"), w2f, j, 4, bW[i])

        ab_i = 0
        o_i = 0
        y_i = 0
        load_block(0)
        for j in range(NBLK):
            i = j % 2
            if j + 1 < NBLK:
                load_block(j + 1)
            for s_ in range(4):
                for kc in range(8):
                    op("pe", lambda e, s_=s_, kc=kc, i=i: e.transpose(
                        out=QT_[:, kc, :], in_=XB[i][:, s_, :].rearrange("p (a k) -> p k a", k=8)[:, kc, :],
                        identity=IDN[:]), reads=[bXB[i], bIDN], writes=[bQT_])
                if s_ % 2 == 0:
                    op("dve", lambda e, s_=s_, i=i: e.tensor_copy(out=XBT[i][:, :, s_ * 128:(s_ + 1) * 128], in_=QT_[:]),
                       reads=[bQT_], writes=[bXBT[i]])
                else:
                    op("act", lambda e, s_=s_, i=i: e.activation(out=XBT[i][:, :, s_ * 128:(s_ + 1) * 128], in_=QT_[:], func=AF.Copy),
                       reads=[bQT_], writes=[bXBT[i]])
            for f in range(4):
                ai = ab_i % 2
                ab_i += 1
                for kc in range(8):
                    op("pe", lambda e, kc=kc, f=f, ai=ai, i=i: e.matmul(
                        out=QA[ai][:, :], lhsT=W1[i][:, kc, :].rearrange("p (a r) -> p r a", r=4)[:, f, :], rhs=XBT[i][:, kc, :],
                        start=(kc == 0), stop=(kc == 7)), reads=[bW[i], bXBT[i]], writes=[bQA[ai]])
                for kc in range(8):
                    op("pe", lambda e, kc=kc, f=f, ai=ai, i=i: e.matmul(
                        out=QB[ai][:, :], lhsT=W3[i][:, kc, :].rearrange("p (a r) -> p r a", r=4)[:, f, :], rhs=XBT[i][:, kc, :],
                        start=(kc == 0), stop=(kc == 7)), reads=[bW[i], bXBT[i]], writes=[bQB[ai]])
                op("act", lambda e, ai=ai: e.activation(out=SA[ai][:], in_=QA[ai][:, :], func=AF.Silu),
                   reads=[bQA[ai]], writes=[bSA[ai]])
                op("dve", lambda e, ai=ai, hb=j % 2, f=f: e.tensor_tensor(out=HID[hb][:, f, :], in0=QB[ai][:, :], in1=SA[ai][:], op=ALU.mult),
                   reads=[bQB[ai], bSA[ai]], writes=[bHID[j % 2]])
            for s_ in range(4):
                yi = y_i % 3
                y_i += 1
                for half in range(2):
                    oi = o_i % 3
                    o_i += 1
                    for f in range(4):
                        op("pe", lambda e, f=f, s_=s_, half=half, oi=oi, i=i, hb=j % 2: e.matmul(
                            out=QO[oi][:, :], lhsT=HID[hb][:, f, s_ * 128:(s_ + 1) * 128],
                            rhs=W2[i][:, f, half * 512:(half + 1) * 512], start=(f == 0), stop=(f == 3)),
                           reads=[bHID[j % 2], bW[i]], writes=[bQO[oi]])
                    if half == 0:
                        op("act", lambda e, oi=oi, yi=yi: e.activation(out=YB[yi][:, 0:512], in_=QO[oi][:, :], func=AF.Copy),
                           reads=[bQO[oi]], writes=[bYB[yi]])
                    else:
                        op("dve", lambda e, oi=oi, yi=yi: e.tensor_copy(out=YB[yi][:, 512:1024], in_=QO[oi][:, :]),
                           reads=[bQO[oi]], writes=[bYB[yi]])
                op("sp", lambda e, yi=yi, j=j, s_=s_: e.dma_start(out=ys_d[j * BS + s_ * 128:j * BS + (s_ + 1) * 128, :],
                                                                 in_=YB[yi][:]), reads=[bYB[yi]], dma=bYB[yi])
        for key, cnt_ in list(sc.dcnt.items()):
            sc._wait("pool", (key, 16 * cnt_))
        for t in range(NCH):
            i = t % 2
            op("sp", lambda e, t=t, i=i: e.dma_start(out=X3L[i][:], in_=out_d[t * 128:(t + 1) * 128, :]), writes=[bX3L[i]], dma=bX3L[i])
            for k in range(2):
                op("pool", lambda e, i=i, k=k, t=t: e.indirect_dma_start(
                    out=Y1[i][:, k, :], out_offset=None, in_=ys_d[:, :],
                    in_offset=bass.IndirectOffsetOnAxis(ap=DPI[:, t, k:k + 1], axis=0)), reads=[bDPI], writes=[bY1[i]], dma=bY1[i])
            op("dve", lambda e, i=i, t=t: e.scalar_tensor_tensor(out=X3L[i][:], in0=Y1[i][:, 0, :], scalar=WAB[:, t, 0:1], in1=X3L[i][:],
                                                                op0=ALU.mult, op1=ALU.add), reads=[bY1[i], bWAB, bX3L[i]], writes=[bX3L[i]])
            op("dve", lambda e, i=i, t=t: e.scalar_tensor_tensor(out=X3L[i][:], in0=Y1[i][:, 1, :], scalar=WAB[:, t, 1:2], in1=X3L[i][:],
                                                                op0=ALU.mult, op1=ALU.add), reads=[bY1[i], bWAB, bX3L[i]], writes=[bX3L[i]])
            op("act", lambda e, i=i: e.activation(out=SC2[:], in_=X3L[i][:], func=AF.Square, accum_out=ST2[:, 0:1]),
               reads=[bX3L[i]], writes=[bSC2, bST2])
            op("dve", lambda e: e.tensor_scalar(out=ST2[:, 0:1], in0=ST2[:, 0:1], scalar1=1.0 / D, scalar2=EPS,
                                                op0=ALU.mult, op1=ALU.add), reads=[bST2], writes=[bST2])
            op("act", lambda e: e.activation(out=ST2[:, 0:1], in_=ST2[:, 0:1], func=AF.Ln), reads=[bST2], writes=[bST2])
            op("act", lambda e: e.activation(out=ST2[:, 0:1], in_=ST2[:, 0:1], func=AF.Exp, scale=-0.5),
               reads=[bST2], writes=[bST2])
            op("dve", lambda e, i=i: e.scalar_tensor_tensor(out=OUT[i][:], in0=X3L[i][:], scalar=ST2[:, 0:1], in1=GFN[:],
                                                           op0=ALU.mult, op1=ALU.mult), reads=[bX3L[i], bST2, bGFN], writes=[bOUT[i]])
            op("sp", lambda e, t=t, i=i: e.dma_start(out=out_d[t * 128:(t + 1) * 128, :], in_=OUT[i][:]), reads=[bOUT[i]], dma=bOUT[i])
        sc.finish("sp")
        sc.emit()
    semstack.__exit__(None, None, None)
    return nc


def _consts():
    ident = np.eye(128, dtype=np.float32)
    s = np.arange(128)[:, None]
    l = np.arange(128)[None, :]
    tri = (s <= l).astype(np.float32)
    ones = np.ones((128, 128), np.float32)
    cbf = np.concatenate([ident, tri, ones], axis=1).astype(ml_dtypes.bfloat16)
    inv_freq = (500000.0 ** (-np.arange(0, 16, 2, dtype=np.float32) / 16)).astype(np.float32)
    invE = np.tile(np.tile(inv_freq, 8)[None, :], (128, 1)).astype(np.float32)
    cf32 = np.concatenate([tri, ones, invE], axis=1).astype(np.float32)
    am = np.zeros((128, NJ * 128), np.float32)
    for j in range(NJ):
        dist = 128 * j + l - s
        m = ((dist >= 0) & (dist <= 128)).astype(np.float32)
        m += ((dist >= 0) & (dist % 4 == 0) & (dist <= 512))
        m += ((dist >= 0) & (dist % 16 == 0) & (dist <= 2048))
        am[:, j * 128:(j + 1) * 128] = np.where(m > 0, 8.0 * np.log(np.maximum(m, 1.0)), -240000.0)
    return cbf, cf32, am.astype(ml_dtypes.bfloat16)


def make_in_maps(inp, S, ncores):
    cbf, cf32, am = _consts()
    f = lambda a: np.ascontiguousarray(np.asarray(a, dtype=np.float32))
    gk = np.zeros((128, 32), np.float32)
    gk[:, 0:8] = f(inp["g_mix"])[0].reshape(8, 128).T
    gk[:, 8:16] = f(inp["g_cross"])[0].reshape(8, 128).T
    gk[:, 16:24] = f(inp["g_mem"])[0].reshape(8, 128).T
    sm = np.zeros((128, 64), np.float32)
    sm[:, 0:4] = f(inp["b_i"])[0][None, :]
    sm[:, 4:8] = f(inp["b_f"])[0][None, :]
    cw = f(inp["conv_w"])[0]
    sm[:, 8:24] = cw.reshape(4, 4, 128).transpose(2, 1, 0).reshape(128, 16)
    sm[:, 24:28] = f(inp["conv_b"])[0].reshape(4, 128).T
    sm[:, 28:32] = f(inp["g_mhn"])[0].reshape(4, 128).T
    sm[:, 32:36] = f(inp["skip_m"])[0].reshape(4, 128).T
    sm[:, 36:40] = f(inp["b_router_g"])[0][None, :]
    sm[:, 40:56] = f(inp["b_router_e"])[0][None, :]
    gff = np.ascontiguousarray(np.tile(f(inp["g_ffn"])[0][None, :], (128, 1)))
    gfin = np.ascontiguousarray(np.tile(f(inp["g_final"])[None, :], (128, 1)))
    w_r = np.ascontiguousarray(np.concatenate([f(inp["w_router_g"])[0], f(inp["w_router_e"])[0]], axis=1))
    sm2 = np.zeros((128, 48), np.float32)
    sm2[:, 32:48] = np.arange(16, dtype=np.float32)[None, :]
    sm2[:, 0:16] = (np.arange(16, dtype=np.float32) * S)[None, :]
    sm2[:, 16] = np.arange(128, dtype=np.float32)
    shared = {
        "sm2": sm2,
        "w_in": f(inp["w_in"])[0], "w_out": f(inp["w_out"])[0], "w_q_m": f(inp["w_q_m"])[0], "w_k_m": f(inp["w_k_m"])[0],
        "w_q_x": f(inp["w_q_x"])[0], "w_kv_x": f(inp["w_kv_x"])[0], "w_o_x": f(inp["w_o_x"])[0], "w_r": w_r,
        "w1": f(inp["w1"])[0], "w3": f(inp["w3"])[0], "w2": f(inp["w2"])[0],
        "gk": gk, "sm": sm, "gff": gff, "gfin": gfin, "cbf": cbf, "cf32": cf32, "amask": am,
    }
    x = np.asarray(inp["x"]); mem = np.asarray(inp["mem"]); pos = np.asarray(inp["positions"])
    maps = []
    for b in range(ncores):
        m = dict(shared)
        m["x"] = np.ascontiguousarray(x[b, :S], dtype=np.float32)
        m["mem"] = np.ascontiguousarray(mem[b], dtype=np.float32)
        m["pos"] = np.ascontiguousarray(pos[b, :S].astype(np.int32).reshape(S // 128, 128).T)
        maps.append(m)
    return maps


def kernel(**inputs):
    S = 8192
    nc = build(S)
    maps = make_in_maps(inputs, S, 8)
    res = run_bass_kernel_spmd(nc, maps, core_ids=list(range(8)))
    return np.stack([np.asarray(r["out"], dtype=np.float32) for r in res.results], axis=0)
```
